# Optimizing a Trainium2 kernel written in Bass

```python
import math
import jax, jax.numpy as jnp
from jax import lax
import numpy as np

D_MODEL = 1024
BATCH = 16
SEQ = 2048
DEPTH = 2

D_MIX = D_MODEL
C_CONV = D_MIX // 4
C_POOL = D_MIX // 4
C_ATTN = D_MIX - C_CONV - C_POOL
DIFF_HEADS = 4
DIFF_HEAD_DIM = C_ATTN // (2 * DIFF_HEADS)
CONV_WIDTH = 31
POOL_WINDOWS = (2, 4, 8, 16)
POOL_GROUP = C_POOL // len(POOL_WINDOWS)
N_IN = 2 * C_CONV + C_POOL + 3 * C_ATTN
N_EXPERT_GROUPS = 4
EXPERTS_PER_GROUP = 4
N_EXPERTS = N_EXPERT_GROUPS * EXPERTS_PER_GROUP
D_EXPERT = D_MODEL // 4
MOE_TOP_K = 2
ATTN_BLOCK = 128
NORM_EPS = 1e-6

kernel_name = "hymba_conv_pool_diffattn_hmoe"


def rms_norm(x, g):
    xf = x.astype(jnp.float32)
    y = xf * lax.rsqrt(jnp.mean(xf * xf, axis=-1, keepdims=True) + NORM_EPS)
    return (y * g.astype(jnp.float32)).astype(x.dtype)


def layer_norm(x, g, b):
    xf = x.astype(jnp.float32)
    mu = jnp.mean(xf, axis=-1, keepdims=True)
    xc = xf - mu
    y = xc * lax.rsqrt(jnp.mean(xc * xc, axis=-1, keepdims=True) + NORM_EPS)
    return (y * g.astype(jnp.float32) + b.astype(jnp.float32)).astype(x.dtype)


def alibi_slopes(n_heads):
    start = 2.0 ** (-8.0 / n_heads)
    return jnp.asarray(np.array([start ** (i + 1) for i in range(n_heads)], dtype=np.float32))


def conv_mixer(a_val, a_gate, w_dw, b_dw, ln_g, ln_b, w_pw, b_pw):
    u = a_val * jax.nn.sigmoid(a_gate)
    y = lax.conv_general_dilated(
        u, w_dw[:, None, :].astype(u.dtype), window_strides=(1,),
        padding=[(CONV_WIDTH - 1, 0)],
        dimension_numbers=("NWC", "WIO", "NWC"),
        feature_group_count=C_CONV) + b_dw
    y = jax.nn.silu(layer_norm(y, ln_g, ln_b))
    return y @ w_pw + b_pw


def pool_mixer(u, w_pool, scale):
    s_len = u.shape[1]
    uf = u.astype(jnp.float32)
    c0 = jnp.pad(lax.cumsum(uf, axis=1), ((0, 0), (1, 0), (0, 0)))
    t = jnp.arange(s_len)
    outs = []
    for gi, win in enumerate(POOL_WINDOWS):
        sl = slice(gi * POOL_GROUP, (gi + 1) * POOL_GROUP)
        lo = jnp.maximum(t + 1 - win, 0)
        window_sum = c0[:, 1:, sl] - c0[:, lo, sl]
        count = jnp.minimum(t + 1, win).astype(jnp.float32)[None, :, None]
        d = (window_sum / count - uf[:, :, sl]).astype(u.dtype)
        outs.append(jnp.einsum("bsc,cd->bsd", d, w_pool[gi]))
    return jnp.concatenate(outs, axis=-1) * scale


def diff_attention(q, k, v, lam, slopes):
    s_len = q.shape[1]
    q = q.transpose(0, 2, 3, 1, 4)
    k = k.transpose(0, 2, 3, 1, 4)
    v = v.transpose(0, 2, 1, 3)
    scale = DIFF_HEAD_DIM ** -0.5
    outs = []
    for blk in range(s_len // ATTN_BLOCK):
        q0 = blk * ATTN_BLOCK
        end = q0 + ATTN_BLOCK
        qb = q[:, :, :, q0:end]
        kb = k[:, :, :, :end]
        vb = v[:, :, :end]
        s = jnp.einsum("bhmqd,bhmkd->bhmqk", qb, kb,
                       preferred_element_type=jnp.float32) * scale
        tq = jnp.arange(q0, end)[:, None]
        tk = jnp.arange(end)[None, :]
        dist = (tq - tk).astype(jnp.float32)
        s = s - slopes[None, :, None, None, None] * dist
        s = jnp.where(tk <= tq, s, -jnp.inf)
        p = jax.nn.softmax(s, axis=-1)
        a = p[:, :, 0] - lam * p[:, :, 1]
        outs.append(jnp.einsum("bhqk,bhkd->bhqd", a.astype(vb.dtype), vb))
    o = jnp.concatenate(outs, axis=2)
    return o.transpose(0, 2, 1, 3)


def hier_moe(h, wg, bg, we, be, w_gate, w_up, w_down):
    b_, s_, d_ = h.shape
    t = h.reshape(-1, d_)
    pg = jax.nn.softmax((t @ wg).astype(jnp.float32) + bg.astype(jnp.float32), axis=-1)
    g_idx = jnp.argmax(pg, axis=-1)
    g_p = jnp.take_along_axis(pg, g_idx[:, None], axis=1)[:, 0]
    le = ((t @ we).astype(jnp.float32) + be.astype(jnp.float32)).reshape(-1, N_EXPERT_GROUPS, EXPERTS_PER_GROUP)
    le = jnp.take_along_axis(le, g_idx[:, None, None], axis=1)[:, 0]
    pe = jax.nn.softmax(le, axis=-1)
    top_p, top_i = lax.top_k(pe, MOE_TOP_K)
    top_p = top_p / jnp.sum(top_p, axis=-1, keepdims=True)
    weights = g_p[:, None] * top_p
    eid = g_idx[:, None] * EXPERTS_PER_GROUP + top_i
    gates = jnp.sum(jax.nn.one_hot(eid, N_EXPERTS, dtype=jnp.float32) * weights[..., None], axis=1)
    gates = gates.astype(t.dtype)
    y = jnp.zeros_like(t)
    for e in range(N_EXPERTS):
        act = jax.nn.silu(t @ w_gate[e]) * (t @ w_up[e]) * gates[:, e:e + 1]
        y = y + act @ w_down[e]
    return y.reshape(b_, s_, d_)


def setup_inputs(seed: int = 0) -> dict:
    key = jax.random.key(seed)
    ks = iter(jax.random.split(key, 40))
    L = DEPTH

    def nrm(shape, scale):
        return jax.random.normal(next(ks), shape, jnp.float32) * scale

    def gain(shape):
        return 1.0 + nrm(shape, 0.02)

    return {
        "x": nrm((BATCH, SEQ, D_MODEL), 1.0),
        "attn_norm_g": gain((L, D_MODEL)),
        "w_in": nrm((L, D_MODEL, N_IN), D_MODEL ** -0.5),
        "conv_w": nrm((L, CONV_WIDTH, C_CONV), CONV_WIDTH ** -0.5),
        "conv_b": nrm((L, C_CONV), 0.02),
        "conv_ln_g": gain((L, C_CONV)),
        "conv_ln_b": nrm((L, C_CONV), 0.02),
        "conv_pw_w": nrm((L, C_CONV, C_CONV), C_CONV ** -0.5),
        "conv_pw_b": nrm((L, C_CONV), 0.02),
        "pool_w": nrm((L, len(POOL_WINDOWS), POOL_GROUP, POOL_GROUP), POOL_GROUP ** -0.5),
        "pool_scale": gain((L, C_POOL)),
        "q_norm_g": gain((L, DIFF_HEAD_DIM)),
        "k_norm_g": gain((L, DIFF_HEAD_DIM)),
        "lambda_q1": nrm((L, DIFF_HEAD_DIM), 0.1),
        "lambda_k1": nrm((L, DIFF_HEAD_DIM), 0.1),
        "lambda_q2": nrm((L, DIFF_HEAD_DIM), 0.1),
        "lambda_k2": nrm((L, DIFF_HEAD_DIM), 0.1),
        "attn_sub_norm_g": gain((L, 2 * DIFF_HEAD_DIM)),
        "w_out": nrm((L, D_MIX, D_MODEL), D_MIX ** -0.5),
        "ffn_norm_g": gain((L, D_MODEL)),
        "router_g_w": nrm((L, D_MODEL, N_EXPERT_GROUPS), D_MODEL ** -0.5),
        "router_g_b": nrm((L, N_EXPERT_GROUPS), 0.01),
        "router_e_w": nrm((L, D_MODEL, N_EXPERTS), D_MODEL ** -0.5),
        "router_e_b": nrm((L, N_EXPERTS), 0.01),
        "w_gate": nrm((L, N_EXPERTS, D_MODEL, D_EXPERT), D_MODEL ** -0.5),
        "w_up": nrm((L, N_EXPERTS, D_MODEL, D_EXPERT), D_MODEL ** -0.5),
        "w_down": nrm((L, N_EXPERTS, D_EXPERT, D_MODEL), D_EXPERT ** -0.5),
    }


def reference(x, attn_norm_g, w_in, conv_w, conv_b, conv_ln_g, conv_ln_b, conv_pw_w, conv_pw_b,
              pool_w, pool_scale, q_norm_g, k_norm_g, lambda_q1, lambda_k1, lambda_q2, lambda_k2,
              attn_sub_norm_g, w_out, ffn_norm_g, router_g_w, router_g_b, router_e_w, router_e_b,
              w_gate, w_up, w_down):
    b_, s_, _ = x.shape
    slopes = alibi_slopes(DIFF_HEADS)
    splits = [C_CONV, 2 * C_CONV, 2 * C_CONV + C_POOL,
              2 * C_CONV + C_POOL + C_ATTN, 2 * C_CONV + C_POOL + 2 * C_ATTN]
    for l in range(DEPTH):
        h = rms_norm(x, attn_norm_g[l])
        z = h @ w_in[l]
        a_val, a_gate, p_in, q, k, v = jnp.split(z, splits, axis=-1)

        y_conv = conv_mixer(a_val, a_gate, conv_w[l], conv_b[l], conv_ln_g[l], conv_ln_b[l],
                            conv_pw_w[l], conv_pw_b[l])
        y_pool = pool_mixer(p_in, pool_w[l], pool_scale[l])

        q = rms_norm(q.reshape(b_, s_, DIFF_HEADS, 2, DIFF_HEAD_DIM), q_norm_g[l])
        k = rms_norm(k.reshape(b_, s_, DIFF_HEADS, 2, DIFF_HEAD_DIM), k_norm_g[l])
        v = v.reshape(b_, s_, DIFF_HEADS, 2 * DIFF_HEAD_DIM)
        lam_init = 0.8 - 0.6 * math.exp(-0.3 * l)
        lam = (jnp.exp(jnp.sum(lambda_q1[l].astype(jnp.float32) * lambda_k1[l].astype(jnp.float32)))
               - jnp.exp(jnp.sum(lambda_q2[l].astype(jnp.float32) * lambda_k2[l].astype(jnp.float32)))
               + lam_init)
        o = diff_attention(q, k, v, lam, slopes)
        o = rms_norm(o, attn_sub_norm_g[l]) * (1.0 - lam_init)
        y_attn = o.reshape(b_, s_, C_ATTN)

        mix = jnp.concatenate([y_conv, y_pool, y_attn], axis=-1)
        x = x + mix @ w_out[l]

        hf = rms_norm(x, ffn_norm_g[l])
        x = x + hier_moe(hf, router_g_w[l], router_g_b[l], router_e_w[l], router_e_b[l],
                         w_gate[l], w_up[l], w_down[l])
    return x
```

```python
import contextlib
import math
import numpy as np
import ml_dtypes
import concourse.bass as bass
import concourse.mybir as mybir
from concourse.bass_utils import run_bass_kernel_spmd

F32 = mybir.dt.float32
BF16 = mybir.dt.bfloat16
ALU = mybir.AluOpType
AF = mybir.ActivationFunctionType
AX = mybir.AxisListType

SEQ = 2048
D = 1024
CH = 512
NCH = SEQ // CH
EPS = 1e-6
SLOPES = [2.0 ** (-2.0 * (i + 1)) for i in range(4)]
LV = 367
NV = 2 * LV + 34
UPAD = 32
PPAD = 16
NEGMASK = -60000.0

COMPUTE = ("pe", "act", "dve", "pool")


class T:
    __slots__ = ("name", "w", "r", "al", "sem", "semv")

    def __init__(self, name):
        self.name = name
        self.w = None
        self.r = []
        self.al = []
        self.sem = None
        self.semv = 0


def alias(a, b):
    a.al.append(b)
    b.al.append(a)


class Sched:
    def __init__(self, nc):
        self.nc = nc
        self.engs = ("pe", "act", "dve", "pool", "sp")
        self.prog = {e: [] for e in self.engs}
        self.cnt = {e: 0 for e in self.engs}
        self.seen = {e: {} for e in self.engs}
        self.semh = {}
        self._ctx = []
        for e in COMPUTE:
            self.new_sem(e)

    def new_sem(self, key):
        cm = self.nc.semaphore("s_" + str(key))
        h = cm.__enter__()
        self._ctx.append(cm)
        self.semh[key] = h
        return h

    def close(self):
        for cm in reversed(self._ctx):
            cm.__exit__(None, None, None)

    def _deps(self, eng, reads, writes):
        deps = {}

        def put(tok):
            if tok is None:
                return
            k, v = tok
            if deps.get(k, 0) < v:
                deps[k] = v
        for t in reads:
            put(t.w)
        for t in writes:
            put(t.w)
            for x in t.r:
                put(x)
            for a in t.al:
                put(a.w)
                for x in a.r:
                    put(x)
        waits = []
        for k, v in deps.items():
            if k == eng and eng == "pe":
                continue
            if self.seen[eng].get(k, 0) < v:
                self.seen[eng][k] = v
                waits.append((k, v))
        return waits

    def op(self, eng, fn, reads=(), writes=()):
        waits = self._deps(eng, reads, writes)
        self.cnt[eng] += 1
        tok = (eng, self.cnt[eng])
        for t in reads:
            t.r.append(tok)
        for t in writes:
            t.w = tok
            t.r = []
        self.prog[eng].append((waits, _record(fn), (eng, 1)))
        return tok

    def dma(self, q, fn, reads=(), writes=(), sem_tile=None):
        st = sem_tile or (writes[0] if writes else reads[0])
        if st.sem is None:
            st.sem = "d_" + st.name
            self.new_sem(st.sem)
        waits = self._deps(q, reads, writes)
        st.semv += 16
        tok = (st.sem, st.semv)
        for t in reads:
            t.r.append(tok)
        for t in writes:
            t.w = tok
            t.r = []
        self.prog[q].append((waits, _record(fn), (st.sem, 16)))
        return tok

    def barrier(self):
        snap = {e: self.cnt[e] for e in COMPUTE}
        for eng in self.engs:
            waits = []
            for k, v in snap.items():
                if k == eng and eng == "pe":
                    continue
                if v > self.seen[eng].get(k, 0):
                    self.seen[eng][k] = v
                    waits.append((k, v))
            if waits:
                self.prog[eng].append((waits, None, None))

    def wait_all(self, eng, tiles):
        waits = self._deps(eng, [], tiles)
        self.prog[eng].append((waits, None, None))

    def emit(self):
        nc = self.nc
        semh = self.semh
        prog = self.prog

        def run(e, name):
            for waits, fn, inc in prog[name]:
                for k, v in waits:
                    e.wait_ge(semh[k], v)
                if fn is not None:
                    ins = None
                    for nm, a, k in fn:
                        ins = getattr(e, nm)(*a, **k)
                    ins.then_inc(semh[inc[0]], inc[1])

        with nc.Block() as block:
            @block.tensor
            def _(e):
                run(e, "pe")

            @block.scalar
            def _(e):
                run(e, "act")

            @block.vector
            def _(e):
                run(e, "dve")

            @block.gpsimd
            def _(e):
                run(e, "pool")

            @block.sync
            def _(e):
                run(e, "sp")


class Rec:
    def __init__(self):
        self.calls = []

    def __getattr__(self, name):
        def f(*a, **k):
            self.calls.append((name, a, k))
            return None
        return f


def _record(fn):
    r = Rec()
    fn(r)
    assert r.calls
    return r.calls


class Rot:
    def __init__(self, items):
        self.items = items
        self.i = 0

    def next(self):
        it = self.items[self.i % len(self.items)]
        self.i += 1
        return it


def build(n_seq=2, n_layer=2, dbg=None):
    nc = bass.Bass("TRN2", target_bir_lowering=False)

    def din(name, shape, dt=F32):
        return nc.dram_tensor(name, shape, dt, kind="ExternalInput").ap()

    x = din("x", [2, SEQ, D])
    w_in = din("w_in", [2, D, 2304])
    w_out = din("w_out", [2, D, D])
    conv_pw = din("conv_pw_w", [2, 256, 256])
    pool_w = din("pool_w", [2, 4, 64, 64])
    w_gate = din("w_gate", [2, 16, D, 256])
    w_up = din("w_up", [2, 16, D, 256])
    w_down = din("w_down", [2, 16, 256, D])
    wr_d = din("wr", [128, 2, 8, 20])
    vecs_d = din("vecs", [128, NV])
    aug_d = din("aug", [2, 4, SEQ])
    cmask_d = din("cmask", [128, 128])
    sel_d = din("sel", [32, 16])
    ident_d = din("ident", [128, 128])
    out = nc.dram_tensor("out", [2, SEQ, D], F32, kind="ExternalOutput").ap()
    dbg_d = None
    if dbg is not None:
        dbg_d = nc.dram_tensor("dbg", [128, 8, SEQ], F32, kind="ExternalOutput").ap()

    S = Sched(nc)
    es = contextlib.ExitStack()

    def sb(name, shape, dt):
        return es.enter_context(nc.sbuf_tensor(name, shape, dt))

    xT = sb("xT", [128, 8, SEQ], F32)
    hT = sb("hT", [128, 8, SEQ], BF16)
    A = sb("A", [128, 18496], BF16)
    dg = sb("dg", [128, 62, 128], BF16)
    wi = sb("wi", [128, 2, 8, 384], BF16)
    wo = sb("wo", [128, 4, 1024], BF16)
    pw = sb("pw", [128, 2, 256], BF16)
    pm = sb("pm", [128, 4, 128], BF16)
    vec = sb("vec", [128, NV], F32)
    wr = sb("wr_s", [128, 2, 8, 20], F32)
    wrg = sb("wrg", [128, 8, 20], F32)
    identf = sb("identf", [128, 128], F32)
    identb = sb("identb", [128, 128], BF16)
    onesb = sb("onesb", [128, 128], BF16)
    maskb = sb("maskb", [128, 128], BF16)
    selc = sb("selc", [32, 16], F32)
    gT = sb("gT", [32, SEQ], BF16)
    cols = sb("cols", [128, 16], F32)
    rcs = sb("rcs", [128, 2, 16], F32)
    lamt = sb("lamt", [128, 2, 64], F32)
    NTF = 8
    NTB = 8
    tf = sb("tf", [128, NTF, 512], F32)
    tb = sb("tb", [128, NTB, 512], BF16)
    sc = sb("sc", [128, 8, 8], F32)
    t16b = sb("t16b", [128, 2, 16], F32)
    rL = sb("rL", [128, 4, 20], F32)
    rA = sb("rA", [128, 4, 4], F32)
    rB = sb("rB", [128, 4, 4], F32)
    rC = sb("rC", [128, 4, 4], F32)
    rD = sb("rD", [128, 4, 4], F32)
    rE = sb("rE", [128, 4, 4], F32)
    rs4 = sb("rs4", [128, 12, 4], F32)
    rG = sb("rG", [128, 4, 16], F32)
    rGb = sb("rGb", [128, 4, 16], BF16)
    rG32 = sb("rG32", [128, 2, 4, 32], F32)
    banks = [es.enter_context(nc.psum_tensor("pb%d" % i, [128, 512], F32)) for i in range(8)]

    mixT = A[:, 0:8192].rearrange("p (c t) -> p c t", c=4)
    QT = A[:, 8192:12288].rearrange("p (m t) -> p m t", m=2)
    KT = A[:, 12288:16384].rearrange("p (m t) -> p m t", m=2)
    V = A[:, 16384:18496].rearrange("p (k d) -> p k d", k=16)
    uT = A[:, 8192:8192 + 2 * (SEQ + UPAD)].rearrange("p (c t) -> p c t", c=2)
    pT = A[:, 12352:12352 + 2 * (SEQ + PPAD)].rearrange("p (c t) -> p c t", c=2)
    gu = [A[:, s * 6144: s * 6144 + 4096].rearrange("p (c n) -> p c n", c=8) for s in range(2)]
    dn = [A[:, s * 6144 + 4096: s * 6144 + 6144].rearrange("p (c n) -> p c n", c=2) for s in range(2)]
    actT = A[:, 12288:14336].rearrange("p (a d t) -> p a d t", a=2, d=2)

    TX = [[T("x%d_%d" % (c, j)) for j in range(NCH)] for c in range(8)]
    TH = [[T("h%d_%d" % (c, j)) for j in range(NCH)] for c in range(8)]
    TM = [[T("m%d_%d" % (c, j)) for j in range(NCH)] for c in range(4)]
    TQ = [[T("q%d_%d" % (m, j)) for j in range(NCH)] for m in range(2)]
    TK = [[T("k%d_%d" % (m, j)) for j in range(NCH)] for m in range(2)]
    TQA, TKA, TVO = T("qa"), T("ka"), T("vo")
    TV = [T("v%d" % j) for j in range(NCH)]
    TU = [[T("u%d_%d" % (c, j)) for j in range(NCH)] for c in range(2)]
    TUP = T("upad")
    TP = [[T("p%d_%d" % (c, j)) for j in range(NCH)] for c in range(2)]
    TPP = T("ppad")
    TDG = [T("dg%d" % c) for c in range(2)]
    TWI = [T("wi%d" % s) for s in range(2)]
    TWO, TPW, TPM = T("wo"), T("pw"), T("pm")
    TGa = [T("ga%d" % s) for s in range(2)]
    TUa = [T("ua%d" % s) for s in range(2)]
    TDa = [T("da%d" % s) for s in range(2)]
    TACT = [[T("act%d_%d" % (a, d)) for d in range(2)] for a in range(2)]
    TVEC, TWR, TWRG, TIDF, TIDB, TONE, TMSK, TSEL = [T(n) for n in "vec wr wrg idf idb one msk sel".split()]
    TGT = [T("gT%d" % j) for j in range(NCH)]
    TCOL, TRCS, TLAM = T("cols"), T("rcs"), T("lamt")
    TTF = [T("tf%d" % i) for i in range(NTF)]
    TTB = [T("tb%d" % i) for i in range(NTB)]
    TSC = [T("sc%d" % i) for i in range(8)]
    TR = T("router")
    TT16 = [T("t16_0"), T("t16_1")]
    TRG = [T("rg32_0"), T("rg32_1")]
    TB = [T("bank%d" % i) for i in range(8)]
    TACC = [[T("acc%d_%d" % (m, j)) for j in range(4)] for m in range(2)]
    accap = [[None] * 4 for _ in range(2)]
    for m in range(2):
        for j in range(4):
            accap[m][j] = banks[3 + j][:, m * 129:(m + 1) * 129]
            TACC[m][j] = TB[3 + j]
    TOUT = T("out")
    TDBG = T("dbg")

    rot_main = Rot([(banks[i], TB[i]) for i in range(4)])
    rot_st = Rot([(banks[i], TB[i]) for i in range(3)])
    rot_y = Rot([(banks[i], TB[i]) for i in (4, 5, 6)])
    rot_ss = Rot([(banks[i], TB[i]) for i in (7, 4, 5, 6)])
    rot_tf = Rot([(tf[:, i, :], TTF[i]) for i in range(4)])
    y32_slots = [[(tf[:, 4 + 2 * p + c, :], TTF[4 + 2 * p + c]) for c in range(2)] for p in range(2)]
    rot_tb = Rot([(tb[:, i, :], TTB[i]) for i in range(4)])
    css_slots = [[(tb[:, 4 + 2 * p + c, :], TTB[4 + 2 * p + c]) for c in range(2)] for p in range(2)]
    rot_sc = Rot([(sc[:, i, :], TSC[i]) for i in range(8)])
    rot_stage = Rot([(tf[:, 2 * i:2 * i + 2, :], [TTF[2 * i], TTF[2 * i + 1]]) for i in range(2)])
    b7, TB7 = banks[7], TB[7]

    def chs(j):
        return slice(j * CH, (j + 1) * CH)

    S.dma("sp", lambda e: e.dma_start(out=vec[:], in_=vecs_d), writes=[TVEC])
    S.dma("sp", lambda e: e.dma_start(out=wr[:], in_=wr_d), writes=[TWR])
    S.dma("sp", lambda e: e.dma_start(out=identf[:], in_=ident_d), writes=[TIDF])
    S.dma("pool", lambda e: e.dma_start(out=identb[:], in_=ident_d), writes=[TIDB])
    S.dma("pool", lambda e: e.dma_start(out=maskb[:], in_=cmask_d), writes=[TMSK])
    S.dma("sp", lambda e: e.dma_start(out=selc[:], in_=sel_d), writes=[TSEL])
    S.op("dve", lambda e: e.memset(onesb[:], 1.0), writes=[TONE])

    def vcol(l, off, n=1):
        return vec[:, l * LV + off: l * LV + off + n]

    def load_x(s, tts=range(16)):
        for tt in tts:
            stg, stt = rot_stage.next()
            S.dma("sp", lambda e, stg=stg, tt=tt: e.dma_start(
                out=stg, in_=x[s, tt * 128:(tt + 1) * 128, :].rearrange("p (a b) -> p a b", a=2)), writes=stt)
            for half in range(2):
                bk, tbk = rot_main.next()

                def tr(e, stg=stg, half=half, bk=bk):
                    for k in range(4):
                        i = e.transpose(bk[:, k * 128:(k + 1) * 128], stg[:, half, k * 128:(k + 1) * 128], identf[:])
                    return i
                S.op("pe", tr, reads=stt + [TIDF], writes=[tbk])
                j = tt // 4
                wt = [TX[c][j] for c in range(4 * half, 4 * half + 4)]
                eng = "dve" if half == 0 else "act"
                dst = xT[:, 4 * half:4 * half + 4, tt * 128:(tt + 1) * 128]
                src = bk[:].rearrange("p (c n) -> p c n", c=4)
                if eng == "dve":
                    S.op("dve", lambda e, dst=dst, src=src: e.tensor_copy(dst, src), reads=[tbk], writes=wt)
                else:
                    S.op("act", lambda e, dst=dst, src=src: e.activation(out=dst, in_=src, func=AF.Copy),
                         reads=[tbk], writes=wt)

    def store_x(s, tts=range(16)):
        for tt in tts:
            stg, stt = rot_stage.next()
            j = tt // 4
            for half in range(2):
                bk, tbk = rot_main.next()

                def tr(e, half=half, bk=bk, tt=tt):
                    for k in range(4):
                        i = e.transpose(bk[:, k * 128:(k + 1) * 128], xT[:, 4 * half + k, tt * 128:(tt + 1) * 128], identf[:])
                    return i
                S.op("pe", tr, reads=[TX[c][j] for c in range(4 * half, 4 * half + 4)] + [TIDF], writes=[tbk])
                if half == 0:
                    S.op("dve", lambda e, stg=stg, bk=bk: e.tensor_copy(stg[:, 0, :], bk[:]), reads=[tbk], writes=[stt[0]])
                else:
                    S.op("act", lambda e, stg=stg, bk=bk: e.activation(out=stg[:, 1, :], in_=bk[:], func=AF.Copy),
                         reads=[tbk], writes=[stt[1]])
            S.dma("sp", lambda e, stg=stg, tt=tt: e.dma_start(
                out=out[s, tt * 128:(tt + 1) * 128, :].rearrange("p (a b) -> p a b", a=2), in_=stg),
                reads=stt, writes=[TOUT], sem_tile=stt[0])

    def rstd_from(bank_ap, tbank, npart, scale):
        r, tr_ = rot_tf.next()
        S.op("act", lambda e: e.activation(out=r[0:npart, :], in_=bank_ap[0:npart, :], func=AF.Ln, bias=EPS, scale=scale),
             reads=[tbank], writes=[tr_])
        S.op("act", lambda e: e.activation(out=r[0:npart, :], in_=r[0:npart, :], func=AF.Exp, scale=-0.5),
             reads=[tr_], writes=[tr_])
        return r, tr_

    def norm(l, goff, j, no_dve=False):
        for half in range(2):
            sqs = []
            for c in range(4 * half, 4 * half + 4):
                q_, tq_ = rot_tb.next()
                if c % 4 == 0:
                    S.op("act", lambda e, q_=q_, c=c: e.activation(out=q_, in_=xT[:, c, chs(j)], func=AF.Square),
                         reads=[TX[c][j]], writes=[tq_])
                elif c % 4 == 2:
                    S.op("pool", lambda e, q_=q_, c=c: e.tensor_tensor(out=q_, in0=xT[:, c, chs(j)], in1=xT[:, c, chs(j)], op=ALU.mult),
                         reads=[TX[c][j]], writes=[tq_])
                elif no_dve:
                    S.op("act", lambda e, q_=q_, c=c: e.activation(out=q_, in_=xT[:, c, chs(j)], func=AF.Square),
                         reads=[TX[c][j]], writes=[tq_])
                else:
                    S.op("dve", lambda e, q_=q_, c=c: e.tensor_tensor(out=q_, in0=xT[:, c, chs(j)], in1=xT[:, c, chs(j)], op=ALU.mult),
                         reads=[TX[c][j]], writes=[tq_])
                sqs.append((q_, tq_))

            def mmf(e, sqs=sqs, half=half):
                for k in range(4):
                    c = 4 * half + k
                    i = e.matmul(b7[:], lhsT=onesb[:], rhs=sqs[k][0], start=(c == 0), stop=(c == 7))
                return i
            S.op("pe", mmf, reads=[t for _, t in sqs] + [TONE], writes=[TB7])
        r, tr_ = rstd_from(b7, TB7, 128, 1.0 / D)
        for c in range(8):
            S.op("dve", lambda e, c=c: e.scalar_tensor_tensor(
                out=hT[:, c, chs(j)], in0=xT[:, c, chs(j)], scalar=vcol(l, goff + c), in1=r,
                op0=ALU.mult, op1=ALU.mult), reads=[TX[c][j], tr_, TVEC], writes=[TH[c][j]])
        return r, tr_

    def load_wi(l, blk, slot):
        if blk < 3:
            c0 = blk * 256
            S.dma("pool", lambda e: e.dma_start(out=wi[:, slot, :, 0:256],
                                                in_=w_in[l, :, c0:c0 + 256].rearrange("(c p) n -> p c n", p=128)),
                  writes=[TWI[slot]])
        else:
            h = blk - 3
            for k, base in enumerate((768, 1280, 1792)):
                c0 = base + h * 128
                S.dma("pool", lambda e, k=k, c0=c0: e.dma_start(
                    out=wi[:, slot, :, k * 128:(k + 1) * 128],
                    in_=w_in[l, :, c0:c0 + 128].rearrange("(c p) n -> p c n", p=128)), writes=[TWI[slot]])

    def layer_consts(l):
        lam_init = 0.8 - 0.6 * math.exp(-0.3 * l)
        for h in range(4):
            S.op("dve", lambda e, h=h: e.tensor_scalar(out=cols[0:64, h:h + 1], in0=vcol(l, 88)[0:64, :],
                                                      scalar1=1.0 / (8.0 * SLOPES[h]), scalar2=None, op0=ALU.mult),
                 reads=[TVEC], writes=[TCOL])
        S.op("dve", lambda e: e.tensor_copy(cols[0:64, 4:5], vcol(l, 89)[0:64, :]), reads=[TVEC], writes=[TCOL])
        iw = vec[:, 2 * LV:2 * LV + 2]
        rc = vec[:, 2 * LV + 2:2 * LV + 34].rearrange("p (c t) -> p c t", c=2)
        S.op("dve", lambda e: e.tensor_tensor(out=cols[:, 5:7], in0=iw, in1=vcol(l, 86, 2), op=ALU.mult),
             reads=[TVEC], writes=[TCOL])
        for cc in range(2):
            S.op("dve", lambda e, cc=cc: e.tensor_scalar(out=rcs[:, cc, :], in0=rc[:, cc, :], scalar1=vcol(l, 86 + cc),
                                                        scalar2=None, op0=ALU.mult), reads=[TVEC], writes=[TRCS])
        S.op("dve", lambda e: e.tensor_scalar(out=cols[:, 7:8], in0=vcol(l, 90), scalar1=1.0 - lam_init, scalar2=None,
                                              op0=ALU.mult), reads=[TVEC], writes=[TCOL])
        lv = vcol(l, 91, 256).rearrange("p (a d) -> p a d", a=4)
        S.op("dve", lambda e: e.tensor_tensor(out=lamt[:, 0, :], in0=lv[:, 0, :], in1=lv[:, 1, :], op=ALU.mult),
             reads=[TVEC], writes=[TLAM])
        S.op("dve", lambda e: e.tensor_tensor(out=lamt[:, 1, :], in0=lv[:, 2, :], in1=lv[:, 3, :], op=ALU.mult),
             reads=[TVEC, TLAM], writes=[TLAM])
        S.op("dve", lambda e: e.reduce_sum(out=cols[:, 9:11], in_=lamt[:], axis=AX.X), reads=[TLAM, TCOL], writes=[TCOL])
        S.op("act", lambda e: e.activation(out=cols[:, 11:13], in_=cols[:, 9:11], func=AF.Exp), reads=[TCOL], writes=[TCOL])
        S.op("dve", lambda e: e.tensor_tensor(out=cols[:, 13:14], in0=cols[:, 12:13], in1=cols[:, 11:12], op=ALU.subtract),
             reads=[TCOL], writes=[TCOL])
        S.op("dve", lambda e: e.tensor_scalar(out=cols[:, 8:9], in0=cols[:, 13:14], scalar1=-lam_init, scalar2=None,
                                              op0=ALU.add), reads=[TCOL], writes=[TCOL])
        for c in range(8):
            S.op("dve", lambda e, c=c: e.tensor_scalar(out=wrg[:, c, :], in0=wr[:, l, c, :], scalar1=vcol(l, 8 + c),
                                                      scalar2=None, op0=ALU.mult), reads=[TWR, TVEC], writes=[TWRG])
        S.dma("pool", lambda e: e.dma_start(out=pw[:], in_=conv_pw[l].rearrange("(c p) n -> p c n", p=128)), writes=[TPW])
        S.op("pool", lambda e: e.memset(pm[:], 0.0), writes=[TPM])
        for cc in range(2):
            glo, ghi = 2 * cc, 2 * cc + 1
            S.dma("pool", lambda e, cc=cc, glo=glo: e.dma_start(out=pm[0:64, 2 * cc, 0:64], in_=pool_w[l, glo]), writes=[TPM])
            S.dma("pool", lambda e, cc=cc, ghi=ghi: e.dma_start(out=pm[64:128, 2 * cc, 64:128], in_=pool_w[l, ghi]), writes=[TPM])
            S.dma("pool", lambda e, cc=cc, ghi=ghi: e.dma_start(out=pm[64:128, 2 * cc + 1, 64:128], in_=pool_w[l, ghi]), writes=[TPM])

    def build_diag(l):
        for cc in range(2):
            for k in range(31):
                S.op("dve", lambda e, cc=cc, k=k: e.tensor_scalar(
                    out=dg[:, cc * 31 + k, :], in0=identb[:], scalar1=vcol(l, 16 + cc * 31 + k), scalar2=None,
                    op0=ALU.mult), reads=[TIDB, TVEC], writes=[TDG[cc]])

    def load_wo(l, part):
        S.dma("pool", lambda e: e.dma_start(out=wo[:], in_=w_out[l, part * 512:(part + 1) * 512, :].rearrange(
            "(c p) n -> p c n", p=128)), writes=[TWO])

    def wout_apply(j):
        for fc in range(8):
            bk, tbk = rot_y.next()

            def mmf(e, bk=bk, fc=fc):
                for c in range(4):
                    i = e.matmul(bk[:], lhsT=wo[:, c, fc * 128:(fc + 1) * 128], rhs=mixT[:, c, chs(j)],
                                 start=(c == 0), stop=(c == 3))
                return i
            S.op("pe", mmf, reads=[TWO] + [TM[c][j] for c in range(4)], writes=[tbk])
            S.op("dve", lambda e, bk=bk, fc=fc: e.tensor_tensor(out=xT[:, fc, chs(j)], in0=bk[:], in1=xT[:, fc, chs(j)],
                                                                op=ALU.add), reads=[tbk, TX[fc][j]], writes=[TX[fc][j]])

    def dump(stage_name, src_ap, src_tiles, bf=False):
        q = "pool" if bf else "sp"
        S.dma(q, lambda e: e.dma_start(out=dbg_d[:, 0:src_ap.shape[1], 0:src_ap.shape[2]], in_=src_ap),
              reads=src_tiles, writes=[TDBG])

    def finish():
        S.wait_all("sp", [TOUT, TDBG])
        S.emit()
        es.close()
        S.close()
        return nc

    wi_prefetched = [False]
    for s in range(n_seq):
        if s == 0:
            load_x(s)
        if dbg == "loadx":
            dump("loadx", xT[:], [t for r in TX for t in r])
            return finish()
        for l in range(n_layer):
            if not wi_prefetched[0]:
                load_wi(l, 1, 1)
                load_wi(l, 0, 0)
            wi_prefetched[0] = False
            layer_consts(l)
            load_wo(l, 0)
            S.op("pool", lambda e: e.memset(uT[:, :, 0:UPAD], 0.0), writes=[TUP])
            S.op("pool", lambda e: e.memset(pT[:, :, 0:PPAD], 0.0), writes=[TPP])
            if dbg == "consts":
                dump("consts", dg[:, :, :].rearrange("p (a k) n -> p a (k n)", a=2), TDG + [TPM, TPW, TCOL, TRCS, TWRG], bf=True)
                return finish()
            for j in range(NCH):
                norm(l, 0, j)
            build_diag(l)
            if dbg == "norm1":
                dump("norm1", hT[:], [t for r in TH for t in r], bf=True)
                return finish()
            for j in range(NCH):
                for cc in range(2):
                    bg_, tbg = rot_main.next()

                    def mmg(e, bg_=bg_, cc=cc, j=j):
                        for c in range(8):
                            i = e.matmul(bg_[:], lhsT=wi[:, 1, c, cc * 128:(cc + 1) * 128], rhs=hT[:, c, chs(j)],
                                         start=(c == 0), stop=(c == 7))
                        return i
                    S.op("pe", mmg, reads=[TWI[1]] + [TH[c][j] for c in range(8)], writes=[tbg])
                    S.op("act", lambda e, bg_=bg_, cc=cc, j=j: e.activation(
                        out=uT[:, cc, UPAD + j * CH:UPAD + (j + 1) * CH], in_=bg_[:], func=AF.Sigmoid),
                        reads=[tbg], writes=[TU[cc][j]])
            load_wi(l, 2, 1)
            for j in range(NCH):
                for cc in range(2):
                    bv, tbv = rot_main.next()

                    def mmv(e, bv=bv, cc=cc, j=j):
                        for c in range(8):
                            i = e.matmul(bv[:], lhsT=wi[:, 0, c, cc * 128:(cc + 1) * 128], rhs=hT[:, c, chs(j)],
                                         start=(c == 0), stop=(c == 7))
                        return i
                    S.op("pe", mmv, reads=[TWI[0]] + [TH[c][j] for c in range(8)], writes=[tbv])
                    usl = uT[:, cc, UPAD + j * CH:UPAD + (j + 1) * CH]
                    S.op("dve", lambda e, bv=bv, usl=usl: e.tensor_tensor(out=usl, in0=bv[:], in1=usl, op=ALU.mult),
                         reads=[tbv, TU[cc][j]], writes=[TU[cc][j]])
            load_wi(l, 3, 0)
            for j in range(NCH):
                for cc in range(2):
                    bp, tbp = rot_main.next()

                    def mmp(e, bp=bp, cc=cc, j=j):
                        for c in range(8):
                            i = e.matmul(bp[:], lhsT=wi[:, 1, c, cc * 128:(cc + 1) * 128], rhs=hT[:, c, chs(j)],
                                         start=(c == 0), stop=(c == 7))
                        return i
                    S.op("pe", mmp, reads=[TWI[1]] + [TH[c][j] for c in range(8)], writes=[tbp])
                    S.op("act", lambda e, bp=bp, cc=cc, j=j: e.activation(
                        out=pT[:, cc, PPAD + j * CH:PPAD + (j + 1) * CH], in_=bp[:], func=AF.Copy),
                        reads=[tbp], writes=[TP[cc][j]])
            if dbg == "inproj":
                dump("inproj", uT[:, :, UPAD:UPAD + SEQ], [t for r in TU + TP for t in r] + [TUP, TPP], bf=True)
                return finish()
            cst = {}

            def convA(j):
                ys = []
                for cc in range(2):
                    bc, tbc = rot_main.next()

                    def mmc(e, bc=bc, cc=cc, j=j):
                        for k in range(31):
                            o = UPAD + j * CH - 30 + k
                            i = e.matmul(bc[:], lhsT=dg[:, cc * 31 + k, :], rhs=uT[:, cc, o:o + CH],
                                         start=(k == 0), stop=(k == 30))
                        return i
                    rd = [TDG[cc], TU[cc][j], TUP] + ([TU[cc][j - 1]] if j > 0 else [])
                    S.op("pe", mmc, reads=rd, writes=[tbc])
                    y32, ty32 = y32_slots[j % 2][cc]
                    S.op("act", lambda e, y32=y32, bc=bc, cc=cc: e.activation(out=y32, in_=bc[:], func=AF.Identity,
                                                                             bias=vcol(l, 78 + cc), scale=1.0),
                         reads=[tbc, TVEC], writes=[ty32])
                    ysq, tysq = rot_tb.next()
                    S.op("act", lambda e, ysq=ysq, bc=bc, cc=cc: e.activation(out=ysq, in_=bc[:], func=AF.Square,
                                                                             bias=vcol(l, 78 + cc), scale=1.0),
                         reads=[tbc, TVEC], writes=[tysq])
                    y16, ty16 = rot_tb.next()
                    S.op("dve", lambda e, y16=y16, y32=y32: e.tensor_copy(y16, y32), reads=[ty32], writes=[ty16])
                    ys.append((y32, ty32, ysq, tysq, y16, ty16))
                cst[j] = {"ys": ys}
                for cc in range(2):
                    wlo, whi = ((2, 4), (8, 16))[cc]
                    ba, tba = rot_main.next()
                    bb, tbb = rot_main.next()
                    rd = [TPM, TP[cc][j], TPP] + ([TP[cc][j - 1]] if j > 0 else [])

                    def mma(e, ba=ba, cc=cc, j=j, wlo=wlo, whi=whi):
                        for k in range(whi):
                            o = PPAD + j * CH - k
                            mi = 2 * cc if k < wlo else 2 * cc + 1
                            i = e.matmul(ba[:], lhsT=pm[:, mi, :], rhs=pT[:, cc, o:o + CH], start=(k == 0), stop=(k == whi - 1))
                        return i
                    S.op("pe", mma, reads=rd, writes=[tba])
                    S.op("pe", lambda e, bb=bb, cc=cc, j=j: e.matmul(bb[:], lhsT=pm[:, 2 * cc, :],
                                                                     rhs=pT[:, cc, PPAD + j * CH:PPAD + (j + 1) * CH],
                                                                     start=True, stop=True), reads=rd, writes=[tbb])
                    bs, tbs = rot_tf.next()
                    S.op("act", lambda e, bs=bs, bb=bb, cc=cc: e.activation(out=bs, in_=bb[:], func=AF.Copy,
                                                                           scale=vcol(l, 86 + cc)),
                         reads=[tbb, TVEC], writes=[tbs])
                    S.op("dve", lambda e, ba=ba, bs=bs, cc=cc, j=j: e.scalar_tensor_tensor(
                        out=mixT[:, 2 + cc, chs(j)], in0=ba[:], scalar=cols[:, 5 + cc:6 + cc], in1=bs,
                        op0=ALU.mult, op1=ALU.subtract), reads=[tba, tbs, TCOL], writes=[TM[2 + cc][j]])
                    if j == 0:
                        t16, tt16 = t16b[:, cc, :], TT16[cc]
                        S.op("dve", lambda e, t16=t16, ba=ba, cc=cc: e.tensor_tensor(out=t16[:, 0:16], in0=ba[:, 0:16],
                                                                                    in1=rcs[:, cc, :], op=ALU.mult),
                             reads=[tba, TRCS], writes=[tt16])
                        S.op("dve", lambda e, t16=t16, bs=bs, cc=cc: e.tensor_tensor(out=mixT[:, 2 + cc, 0:16], in0=t16[:, 0:16],
                                                                                    in1=bs[:, 0:16], op=ALU.subtract),
                             reads=[tt16, tbs], writes=[TM[2 + cc][0]])

            def convB(j):
                ys = cst[j]["ys"]
                bm, tbm = rot_ss.next()
                bx, tbx = rot_ss.next()

                def mmm(e, bm=bm):
                    for cc in range(2):
                        i = e.matmul(bm[:], lhsT=onesb[:], rhs=ys[cc][4], start=(cc == 0), stop=(cc == 1))
                    return i
                S.op("pe", mmm, reads=[TONE, ys[0][5], ys[1][5]], writes=[tbm])

                def mmx(e, bx=bx):
                    for cc in range(2):
                        i = e.matmul(bx[:], lhsT=onesb[:], rhs=ys[cc][2], start=(cc == 0), stop=(cc == 1))
                    return i
                S.op("pe", mmx, reads=[TONE, ys[0][3], ys[1][3]], writes=[tbx])
                mean, tmean = rot_tf.next()
                S.op("dve", lambda e, mean=mean, bm=bm: e.tensor_scalar(out=mean, in0=bm[:], scalar1=1.0 / 256, scalar2=None,
                                                                       op0=ALU.mult), reads=[tbm], writes=[tmean])
                var, tvar = rot_tf.next()
                S.op("dve", lambda e, var=var, mean=mean: e.tensor_tensor(out=var, in0=mean, in1=mean, op=ALU.mult),
                     reads=[tmean], writes=[tvar])
                S.op("dve", lambda e, var=var, bx=bx: e.scalar_tensor_tensor(out=var, in0=bx[:], scalar=1.0 / 256, in1=var,
                                                                            op0=ALU.mult, op1=ALU.subtract),
                     reads=[tbx, tvar], writes=[tvar])
                cst[j]["mv"] = (mean, tmean, var, tvar)

            def convB2(j):
                ys = cst[j]["ys"]
                mean, tmean, var, tvar = cst[j]["mv"]
                S.op("act", lambda e: e.activation(out=var, in_=var, func=AF.Ln, bias=EPS, scale=1.0),
                     reads=[tvar], writes=[tvar])
                S.op("act", lambda e: e.activation(out=var, in_=var, func=AF.Exp, scale=-0.5),
                     reads=[tvar], writes=[tvar])
                css = []
                for cc in range(2):
                    y32, ty32 = ys[cc][0], ys[cc][1]
                    S.op("dve", lambda e, y32=y32: e.tensor_tensor(out=y32, in0=y32, in1=mean, op=ALU.subtract),
                         reads=[ty32, tmean], writes=[ty32])
                    S.op("dve", lambda e, y32=y32: e.tensor_tensor(out=y32, in0=y32, in1=var, op=ALU.mult),
                         reads=[ty32, tvar], writes=[ty32])
                    cs, tcs = css_slots[j % 2][cc]
                    S.op("act", lambda e, cs=cs, y32=y32, cc=cc: e.activation(out=cs, in_=y32, func=AF.Silu,
                                                                             bias=vcol(l, 82 + cc), scale=vcol(l, 80 + cc)),
                         reads=[ty32, TVEC], writes=[tcs])
                    css.append((cs, tcs))
                cst[j]["css"] = css

            def convC(j):
                css = cst[j]["css"]
                for oc in range(2):
                    bo, tbo = rot_main.next()

                    def mmo(e, bo=bo, oc=oc):
                        for cc in range(2):
                            i = e.matmul(bo[:], lhsT=pw[:, cc, oc * 128:(oc + 1) * 128], rhs=css[cc][0],
                                         start=(cc == 0), stop=(cc == 1))
                        return i
                    S.op("pe", mmo, reads=[TPW, css[0][1], css[1][1]], writes=[tbo])
                    S.op("act", lambda e, bo=bo, oc=oc, j=j: e.activation(out=mixT[:, oc, chs(j)], in_=bo[:], func=AF.Identity,
                                                                         bias=vcol(l, 84 + oc), scale=1.0),
                         reads=[tbo, TVEC], writes=[TM[oc][j]])

            for i_ in range(NCH + 1):
                if i_ < NCH:
                    convA(i_)
                if 0 <= i_ - 1 < NCH:
                    convB2(i_ - 1)
                if i_ < NCH:
                    convB(i_)
                if 0 <= i_ - 1 < NCH:
                    convC(i_ - 1)
                    if dbg not in ("convonly", "poolonly"):
                        wout_apply(i_ - 1)
            if dbg in ("convonly", "poolonly"):
                dump(dbg, mixT[:], [t for r in TM for t in r], bf=True)
                return finish()
            if dbg == "convpool" and l == dbg_layer[0]:
                dump("convpool", xT[:], [t for r in TX for t in r])
                return finish()
            S.barrier()
            load_wo(l, 1)
            for m in range(2):
                S.dma("pool", lambda e, m=m: e.dma_start(out=QT[64:68, m, :], in_=aug_d[0]), writes=[TQA])
                S.dma("pool", lambda e, m=m: e.dma_start(out=KT[64:68, m, :], in_=aug_d[1]), writes=[TKA])
            S.op("pool", lambda e: e.memset(V[:, :, 128:129], 1.0), writes=[TVO])
            carry = []

            def finalize2(w_, tw_, rr, trr):
                S.op("act", lambda e: e.activation(out=rr[:, 4:5], in_=rr[:, 3:4], func=AF.Ln, bias=EPS, scale=1.0 / 128),
                     reads=[trr], writes=[trr])
                S.op("act", lambda e: e.activation(out=rr[:, 5:6], in_=rr[:, 4:5], func=AF.Exp, scale=-0.5),
                     reads=[trr], writes=[trr])
                S.op("dve", lambda e: e.tensor_scalar(out=w_[:, 129:257], in0=w_[:, 0:128], scalar1=rr[:, 5:6], scalar2=None,
                                                      op0=ALU.mult), reads=[tw_, trr], writes=[tw_])

            carry_state = [False]

            def emit_carry_a():
                if carry and not carry_state[0]:
                    for (jq, w_, tw_, qcc, rr, trr, hh) in carry:
                        finalize2(w_, tw_, rr, trr)
                    carry_state[0] = True

            def emit_carry():
                emit_carry_a()
                carry_state[0] = False
                for (jq, w_, tw_, qcc, rr, trr, hh) in carry:
                    S.op("pe", lambda e, jq=jq, w_=w_: e.transpose(b7[:, jq * 128:(jq + 1) * 128], w_[:, 129:257], identf[:]),
                         reads=[tw_, TIDF], writes=[TB7])
                    if jq == 3:
                        S.op("act", lambda e, qcc=qcc, hh=hh: e.activation(out=mixT[:, hh, chs(qcc)], in_=b7[:], func=AF.Identity,
                                                                          scale=cols[:, 7:8]), reads=[TB7, TCOL], writes=[TM[hh][qcc]])
                carry.clear()

            emit_carry_prev = emit_carry
            for h in range(4):
                slot = h % 2
                def qk_post(zb, tzb, qk, m, j, h=h):
                    sq, tsq = rot_tb.next()
                    S.op("act", lambda e: e.activation(out=sq[0:64, :], in_=zb[0:64, :], func=AF.Square),
                         reads=[tzb], writes=[tsq])
                    bs_, tbs_ = rot_ss.next()
                    S.op("pe", lambda e: e.matmul(bs_[0:64, :], lhsT=onesb[0:64, 0:64], rhs=sq[0:64, :],
                                                  start=True, stop=True), reads=[tsq, TONE], writes=[tbs_])
                    r, tr_ = rstd_from(bs_, tbs_, 64, 1.0 / 64)
                    dst = (QT if qk == 0 else KT)[0:64, m, chs(j)]
                    dt_ = (TQ if qk == 0 else TK)[m][j]
                    mc = cols[0:64, h:h + 1] if qk == 0 else cols[0:64, 4:5]
                    S.op("dve", lambda e: e.scalar_tensor_tensor(
                        out=dst, in0=zb[0:64, :], scalar=mc, in1=r[0:64, :], op0=ALU.mult, op1=ALU.mult),
                        reads=[tzb, tr_, TCOL], writes=[dt_])
                qk_pend = None
                ngrp = 0
                for j in range(NCH):
                    for qk in range(2):
                        for m in range(2):
                            zb, tzb = rot_main.next()
                            co = qk * 128 + m * 64

                            def mmz(e, zb=zb, co=co, j=j):
                                for c in range(8):
                                    i = e.matmul(zb[0:64, :], lhsT=wi[:, slot, c, co:co + 64], rhs=hT[:, c, chs(j)],
                                                 start=(c == 0), stop=(c == 7))
                                return i
                            S.op("pe", mmz, reads=[TWI[slot]] + [TH[c][j] for c in range(8)], writes=[tzb])
                            if qk_pend is not None:
                                qk_post(*qk_pend)
                            qk_pend = (zb, tzb, qk, m, j)
                            ngrp += 1
                            if ngrp == 2 and carry:
                                emit_carry_a()
                            if ngrp == 4 and carry:
                                emit_carry_prev()
                    vb, tvb = rot_main.next()

                    def mmv2(e, vb=vb, j=j):
                        for tt in range(4):
                            for c in range(8):
                                i = e.matmul(vb[:, tt * 128:(tt + 1) * 128], lhsT=hT[:, c, j * CH + tt * 128:j * CH + (tt + 1) * 128],
                                             rhs=wi[:, slot, c, 256:384], start=(c == 0), stop=(c == 7))
                        return i
                    S.op("pe", mmv2, reads=[TWI[slot]] + [TH[c][j] for c in range(8)], writes=[tvb])
                    S.op("dve", lambda e, vb=vb, j=j: e.tensor_copy(V[:, 4 * j:4 * j + 4, 0:128],
                                                                   vb[:].rearrange("p (k d) -> p k d", k=4)),
                         reads=[tvb], writes=[TV[j]])
                qk_post(*qk_pend)
                if h < 3:
                    load_wi(l, 4 + h, (h + 1) % 2)
                if dbg == "qkv" and l == dbg_layer[0] and h == 0:
                    dump("qkv", A[:, 8192:16384].rearrange("p (c t) -> p c t", c=4), [t for r in TQ + TK for t in r] + [TQA, TKA], bf=True)
                    return finish()
                for qc in range(NCH):
                    nkb = 4 * qc + 4
                    pend = None
                    fin_pending = []
                    chain_pend = None

                    def emit_pv(kb, pts, c0):
                        jq_min = max(0, kb - 4 * qc)
                        for m in range(2):
                            pt, tpt = pts[m]
                            for jq in range(jq_min, 4):
                                a = accap[m][jq]
                                S.op("pe", lambda e, a=a, pt=pt, jq=jq, kb=kb, c0=c0: e.matmul(
                                    a, lhsT=pt[:, jq * 128 - c0:jq * 128 - c0 + 128], rhs=V[:, kb, 0:129],
                                    start=(kb == 0 and m == 0), stop=(kb == 4 * qc + jq),
                                    skip_group_check=True),
                                    reads=[tpt, TV[kb // 4], TVO], writes=[TACC[m][jq]])

                    def fin_copy(jq):
                        bkq = banks[3 + jq]
                        rr, trr = rot_sc.next()
                        w_, tw_ = y32_slots[jq // 2][jq % 2]
                        S.op("dve", lambda e: e.tensor_copy(w_[:, 0:258], bkq[:, 0:258]), reads=[TB[3 + jq]], writes=[tw_])
                        return (jq, w_, tw_, rr, trr)

                    def fin_chain(args, h=h):
                        jq, w_, tw_, rr, trr = args
                        S.op("dve", lambda e: e.reciprocal(out=rr[:, 0:1], in_=w_[:, 128:129]), reads=[tw_], writes=[trr])
                        S.op("dve", lambda e: e.reciprocal(out=rr[:, 1:2], in_=w_[:, 257:258]), reads=[tw_], writes=[trr])
                        S.op("dve", lambda e: e.tensor_tensor(out=rr[:, 2:3], in0=rr[:, 1:2], in1=cols[:, 8:9], op=ALU.mult),
                             reads=[trr, TCOL], writes=[trr])
                        S.op("dve", lambda e: e.tensor_scalar(out=w_[:, 129:257], in0=w_[:, 129:257], scalar1=rr[:, 2:3], scalar2=None,
                                                              op0=ALU.mult), reads=[trr, tw_], writes=[tw_])
                        S.op("dve", lambda e: e.scalar_tensor_tensor(out=w_[:, 0:128], in0=w_[:, 0:128], scalar=rr[:, 0:1],
                                                                     in1=w_[:, 129:257], op0=ALU.mult, op1=ALU.add),
                             reads=[trr, tw_], writes=[tw_])
                        S.op("dve", lambda e: e.tensor_tensor(out=w_[:, 258:386], in0=w_[:, 0:128], in1=w_[:, 0:128],
                                                              op=ALU.mult), reads=[tw_], writes=[tw_])
                        S.op("dve", lambda e: e.reduce_sum(out=rr[:, 3:4], in_=w_[:, 258:386], axis=AX.X), reads=[tw_, trr], writes=[trr])
                        return (jq, w_, tw_, qc, rr, trr, h)

                    for kb in range(nkb):
                        jq_min = max(0, kb - 4 * qc)
                        c0 = jq_min * 128
                        pts = []
                        if kb == 3 and carry:
                            emit_carry_a()
                        if kb == 5 and carry:
                            emit_carry()
                        for m in range(2):
                            st, tst = rot_st.next()
                            rd = [TK[m][kb // 4], TKA, TQ[m][qc], TQA]
                            if kb >= 4 * qc:
                                def mmd(e, st=st, m=m, kb=kb, c0=c0):
                                    e.matmul(st[:, c0:c0 + 128], lhsT=identb[:], rhs=maskb[:], start=True, stop=False)
                                    i = e.matmul(st[:, c0:c0 + 128], lhsT=KT[0:68, m, kb * 128:(kb + 1) * 128],
                                                 rhs=QT[0:68, m, qc * CH + c0:qc * CH + c0 + 128], start=False, stop=True)
                                    if c0 + 128 < CH:
                                        i = e.matmul(st[:, c0 + 128:CH], lhsT=KT[0:68, m, kb * 128:(kb + 1) * 128],
                                                     rhs=QT[0:68, m, qc * CH + c0 + 128:(qc + 1) * CH], start=True, stop=True)
                                    return i
                                S.op("pe", mmd, reads=rd + [TIDB, TMSK], writes=[tst])
                            else:
                                S.op("pe", lambda e, st=st, m=m, kb=kb: e.matmul(
                                    st[:], lhsT=KT[0:68, m, kb * 128:(kb + 1) * 128], rhs=QT[0:68, m, chs(qc)],
                                    start=True, stop=True), reads=rd, writes=[tst])
                            pt, tpt = rot_tb.next()
                            S.op("act", lambda e, pt=pt, st=st, c0=c0: e.activation(out=pt[:, 0:CH - c0], in_=st[:, c0:CH],
                                                                                   func=AF.Exp, scale=SLOPES[h]),
                                 reads=[tst], writes=[tpt])
                            pts.append((pt, tpt))
                        if pend is not None:
                            emit_pv(*pend)
                            kbp = pend[0]
                            if kbp >= 4 * qc:
                                cnew = fin_copy(kbp - 4 * qc)
                                if chain_pend is not None:
                                    fin_pending.append(fin_chain(chain_pend))
                                chain_pend = cnew
                        pend = (kb, pts, c0)
                    emit_pv(*pend)
                    cnew = fin_copy(3)
                    if chain_pend is not None:
                        fin_pending.append(fin_chain(chain_pend))
                    fin_pending.append(fin_chain(cnew))
                    chain_pend = None
                    assert not carry
                    carry.extend(fin_pending)
                    fin_pending.clear()
                if h == 3 or dbg == "attn":
                    emit_carry()
                if dbg == "attn" and l == dbg_layer[0] and h == 0:
                    dump("attn", mixT[:, 0:1, :], [t for t in TM[0]], bf=True)
                    return finish()
            for j in range(NCH):
                wout_apply(j)
            if dbg == "mixer" and l == dbg_layer[0]:
                dump("mixer", xT[:], [t for r in TX for t in r])
                return finish()
            S.barrier()
            def load_expert(e_, slot):
                S.dma("pool", lambda e: e.dma_start(out=gu[slot][:, :, 0:256],
                                                    in_=w_gate[l, e_].rearrange("(c p) n -> p c n", p=128)), writes=[TGa[slot]])
                S.dma("pool", lambda e: e.dma_start(out=gu[slot][:, :, 256:512],
                                                    in_=w_up[l, e_].rearrange("(c p) n -> p c n", p=128)), writes=[TUa[slot]])
                S.dma("pool", lambda e: e.dma_start(out=dn[slot][:],
                                                    in_=w_down[l, e_].rearrange("(c p) n -> p c n", p=128)), writes=[TDa[slot]])
            load_expert(0, 0)
            load_expert(1, 1)
            gt_pend = [None]
            for j in range(NCH):
                r, tr_ = norm(l, 8, j, no_dve=True)
                bk, tbk = rot_main.next()

                def mmr(e, bk=bk, j=j, r=r):
                    for tt in range(4):
                        for c in range(8):
                            e.matmul(bk[:, tt * 32:tt * 32 + 20], lhsT=xT[:, c, j * CH + tt * 128:j * CH + (tt + 1) * 128],
                                     rhs=wrg[:, c, :], start=(c == 0), stop=(c == 7))
                        i = e.matmul(bk[:, tt * 32 + 20:tt * 32 + 21], lhsT=r[0:1, tt * 128:(tt + 1) * 128],
                                     rhs=identf[0:1, 0:1], start=True, stop=True)
                    return i
                S.op("pe", mmr, reads=[TX[c][j] for c in range(8)] + [TWRG, tr_, TIDF], writes=[tbk])
                bv = bk[:, 0:128].rearrange("p (t n) -> p t n", t=4)
                rd, wrt = [TR, TVEC], [TR]

                def R(fn, extra=()):
                    S.op("dve", fn, reads=rd + list(extra), writes=wrt)
                R(lambda e, bv=bv: e.tensor_copy(rs4[:, 0, :], bv[:, :, 20]), [tbk])
                for tt in range(4):
                    R(lambda e, bv=bv, tt=tt: e.scalar_tensor_tensor(out=rL[:, tt, :], in0=bv[:, tt, 0:20], scalar=rs4[:, 0, tt:tt + 1],
                                                                    in1=vcol(l, 347, 20), op0=ALU.mult, op1=ALU.add), [tbk])
                Lg = rL[:, :, 0:4]

                def bc(ap2):
                    return ap2.unsqueeze(2).to_broadcast([128, 4, 4])
                R(lambda e: e.tensor_reduce(out=rs4[:, 1, :], in_=Lg, axis=AX.X, op=ALU.max))
                R(lambda e: e.tensor_tensor(out=rA[:], in0=Lg, in1=bc(rs4[:, 1, :]), op=ALU.is_equal))
                R(lambda e: e.tensor_tensor(out=rB[:], in0=Lg, in1=bc(rs4[:, 1, :]), op=ALU.subtract))
                S.op("act", lambda e: e.activation(out=rB[:], in_=rB[:], func=AF.Exp), reads=rd, writes=wrt)
                R(lambda e: e.reduce_sum(out=rs4[:, 2, :], in_=rB[:], axis=AX.X))
                R(lambda e: e.reciprocal(out=rs4[:, 3, :], in_=rs4[:, 2, :]))
                for g in range(4):
                    le_g = rL[:, :, 4 + 4 * g:8 + 4 * g]
                    if g == 0:
                        R(lambda e, le_g=le_g: e.tensor_tensor(out=rC[:], in0=le_g, in1=bc(rA[:, :, 0]), op=ALU.mult))
                    else:
                        R(lambda e, le_g=le_g, g=g: e.tensor_tensor(out=rD[:], in0=le_g, in1=bc(rA[:, :, g]), op=ALU.mult))
                        R(lambda e: e.tensor_tensor(out=rC[:], in0=rC[:], in1=rD[:], op=ALU.add))
                R(lambda e: e.tensor_reduce(out=rs4[:, 4, :], in_=rC[:], axis=AX.X, op=ALU.max))
                R(lambda e: e.tensor_tensor(out=rD[:], in0=rC[:], in1=bc(rs4[:, 4, :]), op=ALU.is_equal))
                R(lambda e: e.scalar_tensor_tensor(out=rE[:], in0=rD[:], scalar=-1e30, in1=rC[:], op0=ALU.mult, op1=ALU.add))
                R(lambda e: e.tensor_reduce(out=rs4[:, 5, :], in_=rE[:], axis=AX.X, op=ALU.max))
                R(lambda e: e.tensor_tensor(out=rE[:], in0=rE[:], in1=bc(rs4[:, 5, :]), op=ALU.is_equal))
                R(lambda e: e.tensor_tensor(out=rs4[:, 6, :], in0=rs4[:, 5, :], in1=rs4[:, 4, :], op=ALU.subtract))
                S.op("act", lambda e: e.activation(out=rs4[:, 6, :], in_=rs4[:, 6, :], func=AF.Exp), reads=rd, writes=wrt)
                R(lambda e: e.tensor_scalar(out=rs4[:, 7, :], in0=rs4[:, 6, :], scalar1=1.0, scalar2=None, op0=ALU.add))
                R(lambda e: e.reciprocal(out=rs4[:, 7, :], in_=rs4[:, 7, :]))
                R(lambda e: e.tensor_tensor(out=rs4[:, 8, :], in0=rs4[:, 6, :], in1=rs4[:, 7, :], op=ALU.mult))
                R(lambda e: e.tensor_tensor(out=rs4[:, 7, :], in0=rs4[:, 7, :], in1=rs4[:, 3, :], op=ALU.mult))
                R(lambda e: e.tensor_tensor(out=rs4[:, 8, :], in0=rs4[:, 8, :], in1=rs4[:, 3, :], op=ALU.mult))
                R(lambda e: e.tensor_tensor(out=rD[:], in0=rD[:], in1=bc(rs4[:, 7, :]), op=ALU.mult))
                R(lambda e: e.tensor_tensor(out=rE[:], in0=rE[:], in1=bc(rs4[:, 8, :]), op=ALU.mult))
                R(lambda e: e.tensor_tensor(out=rD[:], in0=rD[:], in1=rE[:], op=ALU.add))
                for g in range(4):
                    R(lambda e, g=g: e.tensor_tensor(out=rG[:, :, 4 * g:4 * g + 4], in0=rD[:], in1=bc(rA[:, :, g]), op=ALU.mult))
                R(lambda e: e.tensor_copy(rGb[:], rG[:]))
                gpar = j % 2
                S.op("dve", lambda e, gpar=gpar: e.tensor_copy(rG32[:, gpar, :, 0:16], rGb[:]), reads=rd, writes=[TR, TRG[gpar]])
                S.op("dve", lambda e, gpar=gpar: e.tensor_tensor(out=rG32[:, gpar, :, 16:32], in0=rG[:], in1=rG32[:, gpar, :, 0:16],
                                                                op=ALU.subtract), reads=rd + [TRG[gpar]], writes=[TR, TRG[gpar]])

                def gate_transposes(j=j, gpar=gpar):
                    bt, tbt = rot_main.next()

                    def trg(e):
                        for tt in range(4):
                            i = e.transpose(bt[0:32, tt * 128:(tt + 1) * 128], rG32[:, gpar, tt, :], identf[:])
                        return i
                    S.op("pe", trg, reads=[TRG[gpar], TIDF], writes=[tbt])
                    S.op("act", lambda e: e.activation(out=gT[:, chs(j)], in_=bt[0:32, :], func=AF.Copy),
                         reads=[tbt], writes=[TGT[j]])
                if gt_pend[0] is not None:
                    gt_pend[0]()
                gt_pend[0] = gate_transposes
            gt_pend[0]()
            gt_pend[0] = None
            if dbg == "router" and l == dbg_layer[0]:
                dump("router", gT[:].rearrange("p (c t) -> p c t", c=1), TGT, bf=True)
                return finish()
            pend_down = None

            def emit_down(e_, slot, j, par):
                for fc in range(8):
                    by, tby = rot_y.next()

                    def mmd2(e, by=by, fc=fc):
                        for dc in range(2):
                            i = e.matmul(by[:], lhsT=dn[slot][:, dc, fc * 128:(fc + 1) * 128], rhs=actT[:, par, dc, :],
                                         start=(dc == 0), stop=(dc == 1))
                        return i
                    S.op("pe", mmd2, reads=[TDa[slot], TACT[par][0], TACT[par][1]], writes=[tby])
                    S.op("dve", lambda e, by=by, fc=fc: e.tensor_tensor(out=xT[:, fc, chs(j)], in0=by[:], in1=xT[:, fc, chs(j)],
                                                                        op=ALU.add), reads=[tby, TX[fc][j]], writes=[TX[fc][j]])
            items = [(e_, j) for e_ in range(16) for j in range(NCH)]
            gstate = {}

            def gate_stage1(idx):
                e_, j = items[idx]
                gm, tgm = rot_tb.next()
                S.op("dve", lambda e: e.tensor_scalar(out=gm[0:32, :], in0=gT[0:32, chs(j)], scalar1=selc[:, e_:e_ + 1],
                                                      scalar2=None, op0=ALU.mult), reads=[TSEL, TGT[j]], writes=[tgm])
                gstate[idx] = (gm, tgm)

            def gate_stage2(idx):
                gm, tgm = gstate.pop(idx)
                S.op("pe", lambda e: e.matmul(b7[:], lhsT=onesb[0:32, :], rhs=gm[0:32, :],
                                              start=True, stop=True), reads=[TONE, tgm], writes=[TB7])
                gb, tgb = rot_tf.next()
                S.op("act", lambda e: e.activation(out=gb, in_=b7[:], func=AF.Copy), reads=[TB7], writes=[tgb])
                gstate[("gb", idx)] = (gb, tgb)

            gate_stage1(0)
            gate_stage2(0)
            item = 0
            for e_ in range(16):
                slot = e_ % 2
                for j in range(NCH):
                    par = item % 2
                    idx = item
                    item += 1
                    gb, tgb = gstate.pop(("gb", idx))
                    if e_ == 12 and j == 0 and dbg is None:
                        nl = l + 1 if l + 1 < n_layer else (0 if s + 1 < n_seq else None)
                        if nl is not None:
                            load_wi(nl, 1, 1)
                            load_wi(nl, 0, 0)
                            wi_prefetched[0] = True
                    if idx + 1 < len(items):
                        gate_stage1(idx + 1)
                    for dc in range(2):
                        bA, tbA = rot_main.next()
                        bB, tbB = rot_main.next()

                        def mmgu(e, bA=bA, bB=bB, dc=dc, j=j, slot=slot):
                            for c in range(8):
                                e.matmul(bA[:], lhsT=gu[slot][:, c, dc * 128:(dc + 1) * 128], rhs=hT[:, c, chs(j)],
                                         start=(c == 0), stop=(c == 7))
                            for c in range(8):
                                i = e.matmul(bB[:], lhsT=gu[slot][:, c, 256 + dc * 128:256 + (dc + 1) * 128], rhs=hT[:, c, chs(j)],
                                             start=(c == 0), stop=(c == 7))
                            return i
                        S.op("pe", mmgu, reads=[TGa[slot], TUa[slot]] + [TH[c][j] for c in range(8)], writes=[tbA, tbB])
                        sg_, tsg = rot_tf.next()
                        S.op("act", lambda e, sg_=sg_, bA=bA: e.activation(out=sg_, in_=bA[:], func=AF.Silu), reads=[tbA], writes=[tsg])
                        S.op("dve", lambda e, sg_=sg_, bB=bB: e.tensor_tensor(out=sg_, in0=bB[:], in1=sg_, op=ALU.mult),
                             reads=[tbB, tsg], writes=[tsg])
                        S.op("pool", lambda e, sg_=sg_, gb=gb, par=par, dc=dc: e.tensor_tensor(out=actT[:, par, dc, :], in0=sg_, in1=gb,
                                                                                             op=ALU.mult),
                             reads=[tsg, tgb], writes=[TACT[par][dc]])
                    if idx + 1 < len(items):
                        gate_stage2(idx + 1)
                    if pend_down is not None:
                        emit_down(*pend_down)
                    pend_down = (e_, slot, j, par)
                    if j == NCH - 1 and e_ + 2 < 16:
                        emit_down(*pend_down)
                        pend_down = None
                        load_expert(e_ + 2, slot)
            if pend_down is not None:
                emit_down(*pend_down)
            if dbg == "layer" and l == dbg_layer[0]:
                dump("layer", xT[:], [t for r in TX for t in r])
                return finish()
            S.barrier()
        if s + 1 < n_seq:
            for tt in range(16):
                store_x(s, [tt])
                load_x(s + 1, [tt])
        else:
            store_x(s)
    return finish()


dbg_layer = [0]
aug_b = None


def _build_with_aug(n_seq=2, n_layer=2, dbg=None, layer=0):
    global aug_b
    dbg_layer[0] = layer
    return build(n_seq, n_layer, dbg)


def host_consts():
    t = np.arange(SEQ)
    aug = np.zeros((2, 4, SEQ), np.float32)
    aug[0, 0] = -128.0 * (t // 128)
    aug[0, 1] = -(t % 128).astype(np.float32)
    aug[0, 2] = 1.0
    aug[0, 3] = 1.0
    aug[1, 0] = 1.0
    aug[1, 1] = 1.0
    aug[1, 2] = 128.0 * (t // 128)
    aug[1, 3] = (t % 128).astype(np.float32)
    k = np.arange(128)[:, None]
    q = np.arange(128)[None, :]
    cmask = np.where(k > q, NEGMASK, 0.0).astype(np.float32)
    sel = np.zeros((32, 16), np.float32)
    for kk in range(32):
        sel[kk, kk % 16] = 1.0
    ident = np.eye(128, dtype=np.float32)
    return aug, cmask, sel, ident


def pack_vecs(inp):
    v = np.zeros((128, NV), np.float32)
    for l in range(2):
        b = l * LV
        v[:, b + 0:b + 8] = inp["attn_norm_g"][l].reshape(8, 128).T
        v[:, b + 8:b + 16] = inp["ffn_norm_g"][l].reshape(8, 128).T
        cw = inp["conv_w"][l]
        for cc in range(2):
            v[:, b + 16 + cc * 31:b + 16 + (cc + 1) * 31] = cw[:, cc * 128:(cc + 1) * 128].T
        v[:, b + 78:b + 80] = inp["conv_b"][l].reshape(2, 128).T
        v[:, b + 80:b + 82] = inp["conv_ln_g"][l].reshape(2, 128).T
        v[:, b + 82:b + 84] = inp["conv_ln_b"][l].reshape(2, 128).T
        v[:, b + 84:b + 86] = inp["conv_pw_b"][l].reshape(2, 128).T
        v[:, b + 86:b + 88] = inp["pool_scale"][l].reshape(2, 128).T
        v[0:64, b + 88] = inp["q_norm_g"][l]
        v[0:64, b + 89] = inp["k_norm_g"][l]
        v[:, b + 90] = inp["attn_sub_norm_g"][l]
        for i, nm in enumerate(("lambda_q1", "lambda_k1", "lambda_q2", "lambda_k2")):
            v[:, b + 91 + 64 * i:b + 91 + 64 * (i + 1)] = inp[nm][l][None, :]
        v[:, b + 347:b + 351] = inp["router_g_b"][l][None, :]
        v[:, b + 351:b + 367] = inp["router_e_b"][l][None, :]
    b = 2 * LV
    wins = np.array([[2, 8], [4, 16]], np.float32)
    for cc in range(2):
        v[0:64, b + cc] = 1.0 / wins[0, cc]
        v[64:128, b + cc] = 1.0 / wins[1, cc]
        for tt in range(16):
            v[0:64, b + 2 + cc * 16 + tt] = 1.0 / min(tt + 1, wins[0, cc])
            v[64:128, b + 2 + cc * 16 + tt] = 1.0 / min(tt + 1, wins[1, cc])
    return v


def pack_wr(inp):
    w = np.concatenate([inp["router_g_w"], inp["router_e_w"]], axis=2)
    w = w.reshape(2, 8, 128, 20).transpose(2, 0, 1, 3)
    return np.ascontiguousarray(w)


_NC_CACHE = {}


def make_in_maps(inp, ncores=8):
    inp = {k: np.asarray(v) for k, v in inp.items()}
    aug, cmask, sel, ident = host_consts()
    shared = {
        "w_in": inp["w_in"], "w_out": inp["w_out"], "conv_pw_w": inp["conv_pw_w"], "pool_w": inp["pool_w"],
        "w_gate": inp["w_gate"], "w_up": inp["w_up"], "w_down": inp["w_down"],
        "wr": pack_wr(inp), "vecs": pack_vecs(inp), "aug": aug, "cmask": cmask, "sel": sel, "ident": ident,
    }
    maps = []
    for c in range(ncores):
        m = dict(shared)
        m["x"] = np.ascontiguousarray(inp["x"][2 * c:2 * c + 2])
        maps.append(m)
    return maps


def kernel(**inputs):
    global aug_b
    if "nc" not in _NC_CACHE:
        _NC_CACHE["nc"] = _build_with_aug()
    nc = _NC_CACHE["nc"]
    maps = make_in_maps(inputs)
    res = run_bass_kernel_spmd(nc, maps, core_ids=list(range(8)))
    outs = [np.asarray(r["out"]) for r in res.results]
    return np.concatenate(outs, axis=0).astype(np.float32)
```

```python
import contextlib
import math
import numpy as np
import ml_dtypes
import concourse.bass as bass
import concourse.mybir as mybir
from concourse.bass_utils import run_bass_kernel_spmd

F32 = mybir.dt.float32
BF16 = mybir.dt.bfloat16
ALU = mybir.AluOpType
AF = mybir.ActivationFunctionType
AX = mybir.AxisListType

SEQ = 2048
D = 1024
CH = 512
NCH = SEQ // CH
EPS = 1e-6
SLOPES = [2.0 ** (-2.0 * (i + 1)) for i in range(4)]
LV = 367
NV = 2 * LV + 34
UPAD = 32
PPAD = 16
NEGMASK = -60000.0

COMPUTE = ("pe", "act", "dve", "pool")


class T:
    __slots__ = ("name", "w", "r", "al", "sem", "semv")

    def __init__(self, name):
        self.name = name
        self.w = None
        self.r = []
        self.al = []
        self.sem = None
        self.semv = 0


def alias(a, b):
    a.al.append(b)
    b.al.append(a)


class Sched:
    def __init__(self, nc):
        self.nc = nc
        self.engs = ("pe", "act", "dve", "pool", "sp")
        self.prog = {e: [] for e in self.engs}
        self.cnt = {e: 0 for e in self.engs}
        self.seen = {e: {} for e in self.engs}
        self.semh = {}
        self._ctx = []
        for e in COMPUTE:
            self.new_sem(e)

    def new_sem(self, key):
        cm = self.nc.semaphore("s_" + str(key))
        h = cm.__enter__()
        self._ctx.append(cm)
        self.semh[key] = h
        return h

    def close(self):
        for cm in reversed(self._ctx):
            cm.__exit__(None, None, None)

    def _deps(self, eng, reads, writes):
        deps = {}

        def put(tok):
            if tok is None:
                return
            k, v = tok
            if deps.get(k, 0) < v:
                deps[k] = v
        for t in reads:
            put(t.w)
        for t in writes:
            put(t.w)
            for x in t.r:
                put(x)
            for a in t.al:
                put(a.w)
                for x in a.r:
                    put(x)
        waits = []
        for k, v in deps.items():
            if k == eng and eng == "pe":
                continue
            if self.seen[eng].get(k, 0) < v:
                self.seen[eng][k] = v
                waits.append((k, v))
        return waits

    def op(self, eng, fn, reads=(), writes=()):
        waits = self._deps(eng, reads, writes)
        self.cnt[eng] += 1
        tok = (eng, self.cnt[eng])
        for t in reads:
            t.r.append(tok)
        for t in writes:
            t.w = tok
            t.r = []
        self.prog[eng].append((waits, _record(fn), (eng, 1)))
        return tok

    def dma(self, q, fn, reads=(), writes=(), sem_tile=None):
        st = sem_tile or (writes[0] if writes else reads[0])
        if st.sem is None:
            st.sem = "d_" + st.name
            self.new_sem(st.sem)
        waits = self._deps(q, reads, writes)
        st.semv += 16
        tok = (st.sem, st.semv)
        for t in reads:
            t.r.append(tok)
        for t in writes:
            t.w = tok
            t.r = []
        self.prog[q].append((waits, _record(fn), (st.sem, 16)))
        return tok

    def barrier(self):
        snap = {e: self.cnt[e] for e in COMPUTE}
        for eng in self.engs:
            waits = []
            for k, v in snap.items():
                if k == eng and eng == "pe":
                    continue
                if v > self.seen[eng].get(k, 0):
                    self.seen[eng][k] = v
                    waits.append((k, v))
            if waits:
                self.prog[eng].append((waits, None, None))

    def wait_all(self, eng, tiles):
        waits = self._deps(eng, [], tiles)
        self.prog[eng].append((waits, None, None))

    def emit(self):
        nc = self.nc
        semh = self.semh
        prog = self.prog

        def run(e, name):
            for waits, fn, inc in prog[name]:
                for k, v in waits:
                    e.wait_ge(semh[k], v)
                if fn is not None:
                    ins = None
                    for nm, a, k in fn:
                        ins = getattr(e, nm)(*a, **k)
                    ins.then_inc(semh[inc[0]], inc[1])

        with nc.Block() as block:
            @block.tensor
            def _(e):
                run(e, "pe")

            @block.scalar
            def _(e):
                run(e, "act")

            @block.vector
            def _(e):
                run(e, "dve")

            @block.gpsimd
            def _(e):
                run(e, "pool")

            @block.sync
            def _(e):
                run(e, "sp")


class Rec:
    def __init__(self):
        self.calls = []

    def __getattr__(self, name):
        def f(*a, **k):
            self.calls.append((name, a, k))
            return None
        return f


def _record(fn):
    r = Rec()
    fn(r)
    assert r.calls
    return r.calls


class Rot:
    def __init__(self, items):
        self.items = items
        self.i = 0

    def next(self):
        it = self.items[self.i % len(self.items)]
        self.i += 1
        return it


def build(n_seq=2, n_layer=2, dbg=None):
    nc = bass.Bass("TRN2", target_bir_lowering=False)

    def din(name, shape, dt=F32):
        return nc.dram_tensor(name, shape, dt, kind="ExternalInput").ap()

    x = din("x", [2, SEQ, D])
    w_in = din("w_in", [2, D, 2304])
    w_out = din("w_out", [2, D, D])
    conv_pw = din("conv_pw_w", [2, 256, 256])
    pool_w = din("pool_w", [2, 4, 64, 64])
    w_gate = din("w_gate", [2, 16, D, 256])
    w_up = din("w_up", [2, 16, D, 256])
    w_down = din("w_down", [2, 16, 256, D])
    wr_d = din("wr", [128, 2, 8, 20])
    vecs_d = din("vecs", [128, NV])
    aug_d = din("aug", [2, 4, SEQ])
    cmask_d = din("cmask", [128, 128])
    sel_d = din("sel", [32, 16])
    ident_d = din("ident", [128, 128])
    out = nc.dram_tensor("out", [2, SEQ, D], F32, kind="ExternalOutput").ap()
    dbg_d = None
    if dbg is not None:
        dbg_d = nc.dram_tensor("dbg", [128, 8, SEQ], F32, kind="ExternalOutput").ap()

    S = Sched(nc)
    es = contextlib.ExitStack()

    def sb(name, shape, dt):
        return es.enter_context(nc.sbuf_tensor(name, shape, dt))

    xT = sb("xT", [128, 8, SEQ], F32)
    hT = sb("hT", [128, 8, SEQ], BF16)
    A = sb("A", [128, 18496], BF16)
    dg = sb("dg", [128, 62, 128], BF16)
    wi = sb("wi", [128, 2, 8, 384], BF16)
    wo = sb("wo", [128, 4, 1024], BF16)
    pw = sb("pw", [128, 2, 256], BF16)
    pm = sb("pm", [128, 4, 128], BF16)
    vec = sb("vec", [128, NV], F32)
    wr = sb("wr_s", [128, 2, 8, 20], F32)
    wrg = sb("wrg", [128, 8, 20], F32)
    identf = sb("identf", [128, 128], F32)
    identb = sb("identb", [128, 128], BF16)
    onesb = sb("onesb", [128, 128], BF16)
    maskb = sb("maskb", [128, 128], BF16)
    selc = sb("selc", [32, 16], F32)
    gT = sb("gT", [32, SEQ], BF16)
    cols = sb("cols", [128, 16], F32)
    rcs = sb("rcs", [128, 2, 16], F32)
    lamt = sb("lamt", [128, 2, 64], F32)
    NTF = 8
    NTB = 8
    tf = sb("tf", [128, NTF, 512], F32)
    tb = sb("tb", [128, NTB, 512], BF16)
    sc = sb("sc", [128, 8, 8], F32)
    t16b = sb("t16b", [128, 2, 16], F32)
    rL = sb("rL", [128, 4, 20], F32)
    rA = sb("rA", [128, 4, 4], F32)
    rB = sb("rB", [128, 4, 4], F32)
    rC = sb("rC", [128, 4, 4], F32)
    rD = sb("rD", [128, 4, 4], F32)
    rE = sb("rE", [128, 4, 4], F32)
    rs4 = sb("rs4", [128, 12, 4], F32)
    rG = sb("rG", [128, 4, 16], F32)
    rGb = sb("rGb", [128, 4, 16], BF16)
    rG32 = sb("rG32", [128, 2, 4, 32], F32)
    banks = [es.enter_context(nc.psum_tensor("pb%d" % i, [128, 512], F32)) for i in range(8)]

    mixT = A[:, 0:8192].rearrange("p (c t) -> p c t", c=4)
    QT = A[:, 8192:12288].rearrange("p (m t) -> p m t", m=2)
    KT = A[:, 12288:16384].rearrange("p (m t) -> p m t", m=2)
    V = A[:, 16384:18496].rearrange("p (k d) -> p k d", k=16)
    uT = A[:, 8192:8192 + 2 * (SEQ + UPAD)].rearrange("p (c t) -> p c t", c=2)
    pT = A[:, 12352:12352 + 2 * (SEQ + PPAD)].rearrange("p (c t) -> p c t", c=2)
    gu = [A[:, s * 6144: s * 6144 + 4096].rearrange("p (c n) -> p c n", c=8) for s in range(2)]
    dn = [A[:, s * 6144 + 4096: s * 6144 + 6144].rearrange("p (c n) -> p c n", c=2) for s in range(2)]
    actT = A[:, 12288:14336].rearrange("p (a d t) -> p a d t", a=2, d=2)

    TX = [[T("x%d_%d" % (c, j)) for j in range(NCH)] for c in range(8)]
    TH = [[T("h%d_%d" % (c, j)) for j in range(NCH)] for c in range(8)]
    TM = [[T("m%d_%d" % (c, j)) for j in range(NCH)] for c in range(4)]
    TQ = [[T("q%d_%d" % (m, j)) for j in range(NCH)] for m in range(2)]
    TK = [[T("k%d_%d" % (m, j)) for j in range(NCH)] for m in range(2)]
    TQA, TKA, TVO = T("qa"), T("ka"), T("vo")
    TV = [T("v%d" % j) for j in range(NCH)]
    TU = [[T("u%d_%d" % (c, j)) for j in range(NCH)] for c in range(2)]
    TUP = T("upad")
    TP = [[T("p%d_%d" % (c, j)) for j in range(NCH)] for c in range(2)]
    TPP = T("ppad")
    TDG = [T("dg%d" % c) for c in range(2)]
    TWI = [T("wi%d" % s) for s in range(2)]
    TWO, TPW, TPM = T("wo"), T("pw"), T("pm")
    TGa = [T("ga%d" % s) for s in range(2)]
    TUa = [T("ua%d" % s) for s in range(2)]
    TDa = [T("da%d" % s) for s in range(2)]
    TACT = [[T("act%d_%d" % (a, d)) for d in range(2)] for a in range(2)]
    TVEC, TWR, TWRG, TIDF, TIDB, TONE, TMSK, TSEL = [T(n) for n in "vec wr wrg idf idb one msk sel".split()]
    TGT = [T("gT%d" % j) for j in range(NCH)]
    TCOL, TRCS, TLAM = T("cols"), T("rcs"), T("lamt")
    TTF = [T("tf%d" % i) for i in range(NTF)]
    TTB = [T("tb%d" % i) for i in range(NTB)]
    TSC = [T("sc%d" % i) for i in range(8)]
    TR = T("router")
    TT16 = [T("t16_0"), T("t16_1")]
    TRG = [T("rg32_0"), T("rg32_1")]
    TB = [T("bank%d" % i) for i in range(8)]
    TACC = [[T("acc%d_%d" % (m, j)) for j in range(4)] for m in range(2)]
    accap = [[None] * 4 for _ in range(2)]
    for m in range(2):
        for j in range(4):
            accap[m][j] = banks[3 + j][:, m * 129:(m + 1) * 129]
            TACC[m][j] = TB[3 + j]
    TOUT = T("out")
    TOUTS = [[T("out%d_%d" % (s_, t_)) for t_ in range(16)] for s_ in range(2)]
    TDBG = T("dbg")

    rot_main = Rot([(banks[i], TB[i]) for i in range(4)])
    rot_st = Rot([(banks[i], TB[i]) for i in range(3)])
    rot_y = Rot([(banks[i], TB[i]) for i in (4, 5, 6)])
    rot_ss = Rot([(banks[i], TB[i]) for i in (7, 4, 5, 6)])
    rot_tf = Rot([(tf[:, i, :], TTF[i]) for i in range(4)])
    y32_slots = [[(tf[:, 4 + 2 * p + c, :], TTF[4 + 2 * p + c]) for c in range(2)] for p in range(2)]
    rot_tb = Rot([(tb[:, i, :], TTB[i]) for i in range(4)])
    css_slots = [[(tb[:, 4 + 2 * p + c, :], TTB[4 + 2 * p + c]) for c in range(2)] for p in range(2)]
    rot_sc = Rot([(sc[:, i, :], TSC[i]) for i in range(8)])
    rot_stage = Rot([(tf[:, 2 * i:2 * i + 2, :], [TTF[2 * i], TTF[2 * i + 1]]) for i in range(2)])
    b7, TB7 = banks[7], TB[7]

    def chs(j):
        return slice(j * CH, (j + 1) * CH)

    S.dma("sp", lambda e: e.dma_start(out=vec[:], in_=vecs_d), writes=[TVEC])
    S.dma("sp", lambda e: e.dma_start(out=wr[:], in_=wr_d), writes=[TWR])
    S.dma("sp", lambda e: e.dma_start(out=identf[:], in_=ident_d), writes=[TIDF])
    S.dma("pool", lambda e: e.dma_start(out=identb[:], in_=ident_d), writes=[TIDB])
    S.dma("pool", lambda e: e.dma_start(out=maskb[:], in_=cmask_d), writes=[TMSK])
    S.dma("sp", lambda e: e.dma_start(out=selc[:], in_=sel_d), writes=[TSEL])
    S.op("dve", lambda e: e.memset(onesb[:], 1.0), writes=[TONE])

    def vcol(l, off, n=1):
        return vec[:, l * LV + off: l * LV + off + n]

    def load_x(s, tts=range(16)):
        for tt in tts:
            stg, stt = rot_stage.next()
            S.dma("sp", lambda e, stg=stg, tt=tt: e.dma_start(
                out=stg, in_=x[s, tt * 128:(tt + 1) * 128, :].rearrange("p (a b) -> p a b", a=2)), writes=stt)
            for half in range(2):
                bk, tbk = rot_main.next()

                def tr(e, stg=stg, half=half, bk=bk):
                    for k in range(4):
                        i = e.transpose(bk[:, k * 128:(k + 1) * 128], stg[:, half, k * 128:(k + 1) * 128], identf[:])
                    return i
                S.op("pe", tr, reads=stt + [TIDF], writes=[tbk])
                j = tt // 4
                wt = [TX[c][j] for c in range(4 * half, 4 * half + 4)]
                eng = "dve" if half == 0 else "act"
                dst = xT[:, 4 * half:4 * half + 4, tt * 128:(tt + 1) * 128]
                src = bk[:].rearrange("p (c n) -> p c n", c=4)
                if eng == "dve":
                    S.op("dve", lambda e, dst=dst, src=src: e.tensor_copy(dst, src), reads=[tbk], writes=wt)
                else:
                    S.op("act", lambda e, dst=dst, src=src: e.activation(out=dst, in_=src, func=AF.Copy),
                         reads=[tbk], writes=wt)

    def store_x(s, tts=range(16)):
        for tt in tts:
            stg, stt = rot_stage.next()
            j = tt // 4
            for half in range(2):
                bk, tbk = rot_main.next()

                def tr(e, half=half, bk=bk, tt=tt):
                    for k in range(4):
                        i = e.transpose(bk[:, k * 128:(k + 1) * 128], xT[:, 4 * half + k, tt * 128:(tt + 1) * 128], identf[:])
                    return i
                S.op("pe", tr, reads=[TX[c][j] for c in range(4 * half, 4 * half + 4)] + [TIDF], writes=[tbk])
                if half == 0:
                    S.op("dve", lambda e, stg=stg, bk=bk: e.tensor_copy(stg[:, 0, :], bk[:]), reads=[tbk], writes=[stt[0]])
                else:
                    S.op("act", lambda e, stg=stg, bk=bk: e.activation(out=stg[:, 1, :], in_=bk[:], func=AF.Copy),
                         reads=[tbk], writes=[stt[1]])
            S.dma("sp", lambda e, stg=stg, tt=tt: e.dma_start(
                out=out[s, tt * 128:(tt + 1) * 128, :].rearrange("p (a b) -> p a b", a=2), in_=stg),
                reads=stt, writes=[TOUTS[s][tt]], sem_tile=stt[0])

    def rstd_from(bank_ap, tbank, npart, scale):
        r, tr_ = rot_tf.next()
        S.op("act", lambda e: e.activation(out=r[0:npart, :], in_=bank_ap[0:npart, :], func=AF.Ln, bias=EPS, scale=scale),
             reads=[tbank], writes=[tr_])
        S.op("act", lambda e: e.activation(out=r[0:npart, :], in_=r[0:npart, :], func=AF.Exp, scale=-0.5),
             reads=[tr_], writes=[tr_])
        return r, tr_

    def norm(l, goff, j, no_dve=False):
        for half in range(2):
            sqs = []
            for c in range(4 * half, 4 * half + 4):
                q_, tq_ = rot_tb.next()
                if c % 4 == 0:
                    S.op("act", lambda e, q_=q_, c=c: e.activation(out=q_, in_=xT[:, c, chs(j)], func=AF.Square),
                         reads=[TX[c][j]], writes=[tq_])
                elif c % 4 == 2:
                    S.op("pool", lambda e, q_=q_, c=c: e.tensor_tensor(out=q_, in0=xT[:, c, chs(j)], in1=xT[:, c, chs(j)], op=ALU.mult),
                         reads=[TX[c][j]], writes=[tq_])
                elif no_dve:
                    S.op("act", lambda e, q_=q_, c=c: e.activation(out=q_, in_=xT[:, c, chs(j)], func=AF.Square),
                         reads=[TX[c][j]], writes=[tq_])
                else:
                    S.op("dve", lambda e, q_=q_, c=c: e.tensor_tensor(out=q_, in0=xT[:, c, chs(j)], in1=xT[:, c, chs(j)], op=ALU.mult),
                         reads=[TX[c][j]], writes=[tq_])
                sqs.append((q_, tq_))

            def mmf(e, sqs=sqs, half=half):
                for k in range(4):
                    c = 4 * half + k
                    i = e.matmul(b7[:], lhsT=onesb[:], rhs=sqs[k][0], start=(c == 0), stop=(c == 7))
                return i
            S.op("pe", mmf, reads=[t for _, t in sqs] + [TONE], writes=[TB7])
        r, tr_ = rstd_from(b7, TB7, 128, 1.0 / D)
        for c in range(8):
            S.op("dve", lambda e, c=c: e.scalar_tensor_tensor(
                out=hT[:, c, chs(j)], in0=xT[:, c, chs(j)], scalar=vcol(l, goff + c), in1=r,
                op0=ALU.mult, op1=ALU.mult), reads=[TX[c][j], tr_, TVEC], writes=[TH[c][j]])
        return r, tr_

    def load_wi(l, blk, slot):
        if blk < 3:
            c0 = blk * 256
            S.dma("pool", lambda e: e.dma_start(out=wi[:, slot, :, 0:256],
                                                in_=w_in[l, :, c0:c0 + 256].rearrange("(c p) n -> p c n", p=128)),
                  writes=[TWI[slot]])
        else:
            h = blk - 3
            for k, base in enumerate((768, 1280, 1792)):
                c0 = base + h * 128
                S.dma("pool", lambda e, k=k, c0=c0: e.dma_start(
                    out=wi[:, slot, :, k * 128:(k + 1) * 128],
                    in_=w_in[l, :, c0:c0 + 128].rearrange("(c p) n -> p c n", p=128)), writes=[TWI[slot]])

    def layer_consts(l):
        lam_init = 0.8 - 0.6 * math.exp(-0.3 * l)
        for h in range(4):
            S.op("dve", lambda e, h=h: e.tensor_scalar(out=cols[0:64, h:h + 1], in0=vcol(l, 88)[0:64, :],
                                                      scalar1=1.0 / (8.0 * SLOPES[h]), scalar2=None, op0=ALU.mult),
                 reads=[TVEC], writes=[TCOL])
        S.op("dve", lambda e: e.tensor_copy(cols[0:64, 4:5], vcol(l, 89)[0:64, :]), reads=[TVEC], writes=[TCOL])
        iw = vec[:, 2 * LV:2 * LV + 2]
        rc = vec[:, 2 * LV + 2:2 * LV + 34].rearrange("p (c t) -> p c t", c=2)
        S.op("dve", lambda e: e.tensor_tensor(out=cols[:, 5:7], in0=iw, in1=vcol(l, 86, 2), op=ALU.mult),
             reads=[TVEC], writes=[TCOL])
        for cc in range(2):
            S.op("dve", lambda e, cc=cc: e.tensor_scalar(out=rcs[:, cc, :], in0=rc[:, cc, :], scalar1=vcol(l, 86 + cc),
                                                        scalar2=None, op0=ALU.mult), reads=[TVEC], writes=[TRCS])
        S.op("dve", lambda e: e.tensor_scalar(out=cols[:, 7:8], in0=vcol(l, 90), scalar1=1.0 - lam_init, scalar2=None,
                                              op0=ALU.mult), reads=[TVEC], writes=[TCOL])
        lv = vcol(l, 91, 256).rearrange("p (a d) -> p a d", a=4)
        S.op("dve", lambda e: e.tensor_tensor(out=lamt[:, 0, :], in0=lv[:, 0, :], in1=lv[:, 1, :], op=ALU.mult),
             reads=[TVEC], writes=[TLAM])
        S.op("dve", lambda e: e.tensor_tensor(out=lamt[:, 1, :], in0=lv[:, 2, :], in1=lv[:, 3, :], op=ALU.mult),
             reads=[TVEC, TLAM], writes=[TLAM])
        S.op("dve", lambda e: e.reduce_sum(out=cols[:, 9:11], in_=lamt[:], axis=AX.X), reads=[TLAM, TCOL], writes=[TCOL])
        S.op("act", lambda e: e.activation(out=cols[:, 11:13], in_=cols[:, 9:11], func=AF.Exp), reads=[TCOL], writes=[TCOL])
        S.op("dve", lambda e: e.tensor_tensor(out=cols[:, 13:14], in0=cols[:, 12:13], in1=cols[:, 11:12], op=ALU.subtract),
             reads=[TCOL], writes=[TCOL])
        S.op("dve", lambda e: e.tensor_scalar(out=cols[:, 8:9], in0=cols[:, 13:14], scalar1=-lam_init, scalar2=None,
                                              op0=ALU.add), reads=[TCOL], writes=[TCOL])
        for c in range(8):
            S.op("dve", lambda e, c=c: e.tensor_scalar(out=wrg[:, c, :], in0=wr[:, l, c, :], scalar1=vcol(l, 8 + c),
                                                      scalar2=None, op0=ALU.mult), reads=[TWR, TVEC], writes=[TWRG])
        S.dma("pool", lambda e: e.dma_start(out=pw[:], in_=conv_pw[l].rearrange("(c p) n -> p c n", p=128)), writes=[TPW])
        S.op("pool", lambda e: e.memset(pm[:], 0.0), writes=[TPM])
        for cc in range(2):
            glo, ghi = 2 * cc, 2 * cc + 1
            S.dma("pool", lambda e, cc=cc, glo=glo: e.dma_start(out=pm[0:64, 2 * cc, 0:64], in_=pool_w[l, glo]), writes=[TPM])
            S.dma("pool", lambda e, cc=cc, ghi=ghi: e.dma_start(out=pm[64:128, 2 * cc, 64:128], in_=pool_w[l, ghi]), writes=[TPM])
            S.dma("pool", lambda e, cc=cc, ghi=ghi: e.dma_start(out=pm[64:128, 2 * cc + 1, 64:128], in_=pool_w[l, ghi]), writes=[TPM])

    def build_diag(l):
        for cc in range(2):
            for k in range(31):
                S.op("dve", lambda e, cc=cc, k=k: e.tensor_scalar(
                    out=dg[:, cc * 31 + k, :], in0=identb[:], scalar1=vcol(l, 16 + cc * 31 + k), scalar2=None,
                    op0=ALU.mult), reads=[TIDB, TVEC], writes=[TDG[cc]])

    def load_wo(l, part):
        S.dma("pool", lambda e: e.dma_start(out=wo[:], in_=w_out[l, part * 512:(part + 1) * 512, :].rearrange(
            "(c p) n -> p c n", p=128)), writes=[TWO])

    def wout_apply(j):
        for fc in range(8):
            bk, tbk = rot_y.next()

            def mmf(e, bk=bk, fc=fc):
                for c in range(4):
                    i = e.matmul(bk[:], lhsT=wo[:, c, fc * 128:(fc + 1) * 128], rhs=mixT[:, c, chs(j)],
                                 start=(c == 0), stop=(c == 3))
                return i
            S.op("pe", mmf, reads=[TWO] + [TM[c][j] for c in range(4)], writes=[tbk])
            S.op("dve", lambda e, bk=bk, fc=fc: e.tensor_tensor(out=xT[:, fc, chs(j)], in0=bk[:], in1=xT[:, fc, chs(j)],
                                                                op=ALU.add), reads=[tbk, TX[fc][j]], writes=[TX[fc][j]])

    def dump(stage_name, src_ap, src_tiles, bf=False):
        q = "pool" if bf else "sp"
        S.dma(q, lambda e: e.dma_start(out=dbg_d[:, 0:src_ap.shape[1], 0:src_ap.shape[2]], in_=src_ap),
              reads=src_tiles, writes=[TDBG])

    def finish():
        S.wait_all("sp", [TOUT, TDBG] + [t for r in TOUTS for t in r])
        S.emit()
        es.close()
        S.close()
        return nc

    wi_prefetched = [False]
    for s in range(n_seq):
        if s == 0:
            load_x(s)
        if dbg == "loadx":
            dump("loadx", xT[:], [t for r in TX for t in r])
            return finish()
        for l in range(n_layer):
            if not wi_prefetched[0]:
                load_wi(l, 1, 1)
                load_wi(l, 0, 0)
            wi_prefetched[0] = False
            layer_consts(l)
            load_wo(l, 0)
            S.op("pool", lambda e: e.memset(uT[:, :, 0:UPAD], 0.0), writes=[TUP])
            S.op("pool", lambda e: e.memset(pT[:, :, 0:PPAD], 0.0), writes=[TPP])
            if dbg == "consts":
                dump("consts", dg[:, :, :].rearrange("p (a k) n -> p a (k n)", a=2), TDG + [TPM, TPW, TCOL, TRCS, TWRG], bf=True)
                return finish()
            for j in range(NCH):
                norm(l, 0, j)
            build_diag(l)
            if dbg == "norm1":
                dump("norm1", hT[:], [t for r in TH for t in r], bf=True)
                return finish()
            for j in range(NCH):
                for cc in range(2):
                    bg_, tbg = rot_main.next()

                    def mmg(e, bg_=bg_, cc=cc, j=j):
                        for c in range(8):
                            i = e.matmul(bg_[:], lhsT=wi[:, 1, c, cc * 128:(cc + 1) * 128], rhs=hT[:, c, chs(j)],
                                         start=(c == 0), stop=(c == 7))
                        return i
                    S.op("pe", mmg, reads=[TWI[1]] + [TH[c][j] for c in range(8)], writes=[tbg])
                    S.op("act", lambda e, bg_=bg_, cc=cc, j=j: e.activation(
                        out=uT[:, cc, UPAD + j * CH:UPAD + (j + 1) * CH], in_=bg_[:], func=AF.Sigmoid),
                        reads=[tbg], writes=[TU[cc][j]])
            load_wi(l, 2, 1)
            for j in range(NCH):
                for cc in range(2):
                    bv, tbv = rot_main.next()

                    def mmv(e, bv=bv, cc=cc, j=j):
                        for c in range(8):
                            i = e.matmul(bv[:], lhsT=wi[:, 0, c, cc * 128:(cc + 1) * 128], rhs=hT[:, c, chs(j)],
                                         start=(c == 0), stop=(c == 7))
                        return i
                    S.op("pe", mmv, reads=[TWI[0]] + [TH[c][j] for c in range(8)], writes=[tbv])
                    usl = uT[:, cc, UPAD + j * CH:UPAD + (j + 1) * CH]
                    S.op("dve", lambda e, bv=bv, usl=usl: e.tensor_tensor(out=usl, in0=bv[:], in1=usl, op=ALU.mult),
                         reads=[tbv, TU[cc][j]], writes=[TU[cc][j]])
            load_wi(l, 3, 0)
            for j in range(NCH):
                for cc in range(2):
                    bp, tbp = rot_main.next()

                    def mmp(e, bp=bp, cc=cc, j=j):
                        for c in range(8):
                            i = e.matmul(bp[:], lhsT=wi[:, 1, c, cc * 128:(cc + 1) * 128], rhs=hT[:, c, chs(j)],
                                         start=(c == 0), stop=(c == 7))
                        return i
                    S.op("pe", mmp, reads=[TWI[1]] + [TH[c][j] for c in range(8)], writes=[tbp])
                    S.op("act", lambda e, bp=bp, cc=cc, j=j: e.activation(
                        out=pT[:, cc, PPAD + j * CH:PPAD + (j + 1) * CH], in_=bp[:], func=AF.Copy),
                        reads=[tbp], writes=[TP[cc][j]])
            if dbg == "inproj":
                dump("inproj", uT[:, :, UPAD:UPAD + SEQ], [t for r in TU + TP for t in r] + [TUP, TPP], bf=True)
                return finish()
            cst = {}

            def convA(j):
                ys = []
                for cc in range(2):
                    bc, tbc = rot_main.next()

                    def mmc(e, bc=bc, cc=cc, j=j):
                        for k in range(31):
                            o = UPAD + j * CH - 30 + k
                            i = e.matmul(bc[:], lhsT=dg[:, cc * 31 + k, :], rhs=uT[:, cc, o:o + CH],
                                         start=(k == 0), stop=(k == 30))
                        return i
                    rd = [TDG[cc], TU[cc][j], TUP] + ([TU[cc][j - 1]] if j > 0 else [])
                    S.op("pe", mmc, reads=rd, writes=[tbc])
                    y32, ty32 = y32_slots[j % 2][cc]
                    S.op("act", lambda e, y32=y32, bc=bc, cc=cc: e.activation(out=y32, in_=bc[:], func=AF.Identity,
                                                                             bias=vcol(l, 78 + cc), scale=1.0),
                         reads=[tbc, TVEC], writes=[ty32])
                    ysq, tysq = rot_tb.next()
                    S.op("act", lambda e, ysq=ysq, bc=bc, cc=cc: e.activation(out=ysq, in_=bc[:], func=AF.Square,
                                                                             bias=vcol(l, 78 + cc), scale=1.0),
                         reads=[tbc, TVEC], writes=[tysq])
                    y16, ty16 = rot_tb.next()
                    S.op("dve", lambda e, y16=y16, y32=y32: e.tensor_copy(y16, y32), reads=[ty32], writes=[ty16])
                    ys.append((y32, ty32, ysq, tysq, y16, ty16))
                cst[j] = {"ys": ys}
                for cc in range(2):
                    wlo, whi = ((2, 4), (8, 16))[cc]
                    ba, tba = rot_main.next()
                    bb, tbb = rot_main.next()
                    rd = [TPM, TP[cc][j], TPP] + ([TP[cc][j - 1]] if j > 0 else [])

                    def mma(e, ba=ba, cc=cc, j=j, wlo=wlo, whi=whi):
                        for k in range(whi):
                            o = PPAD + j * CH - k
                            mi = 2 * cc if k < wlo else 2 * cc + 1
                            i = e.matmul(ba[:], lhsT=pm[:, mi, :], rhs=pT[:, cc, o:o + CH], start=(k == 0), stop=(k == whi - 1))
                        return i
                    S.op("pe", mma, reads=rd, writes=[tba])
                    S.op("pe", lambda e, bb=bb, cc=cc, j=j: e.matmul(bb[:], lhsT=pm[:, 2 * cc, :],
                                                                     rhs=pT[:, cc, PPAD + j * CH:PPAD + (j + 1) * CH],
                                                                     start=True, stop=True), reads=rd, writes=[tbb])
                    bs, tbs = rot_tf.next()
                    S.op("act", lambda e, bs=bs, bb=bb, cc=cc: e.activation(out=bs, in_=bb[:], func=AF.Copy,
                                                                           scale=vcol(l, 86 + cc)),
                         reads=[tbb, TVEC], writes=[tbs])
                    S.op("dve", lambda e, ba=ba, bs=bs, cc=cc, j=j: e.scalar_tensor_tensor(
                        out=mixT[:, 2 + cc, chs(j)], in0=ba[:], scalar=cols[:, 5 + cc:6 + cc], in1=bs,
                        op0=ALU.mult, op1=ALU.subtract), reads=[tba, tbs, TCOL], writes=[TM[2 + cc][j]])
                    if j == 0:
                        t16, tt16 = t16b[:, cc, :], TT16[cc]
                        S.op("dve", lambda e, t16=t16, ba=ba, cc=cc: e.tensor_tensor(out=t16[:, 0:16], in0=ba[:, 0:16],
                                                                                    in1=rcs[:, cc, :], op=ALU.mult),
                             reads=[tba, TRCS], writes=[tt16])
                        S.op("dve", lambda e, t16=t16, bs=bs, cc=cc: e.tensor_tensor(out=mixT[:, 2 + cc, 0:16], in0=t16[:, 0:16],
                                                                                    in1=bs[:, 0:16], op=ALU.subtract),
                             reads=[tt16, tbs], writes=[TM[2 + cc][0]])

            def convB(j):
                ys = cst[j]["ys"]
                bm, tbm = rot_ss.next()
                bx, tbx = rot_ss.next()

                def mmm(e, bm=bm):
                    for cc in range(2):
                        i = e.matmul(bm[:], lhsT=onesb[:], rhs=ys[cc][4], start=(cc == 0), stop=(cc == 1))
                    return i
                S.op("pe", mmm, reads=[TONE, ys[0][5], ys[1][5]], writes=[tbm])

                def mmx(e, bx=bx):
                    for cc in range(2):
                        i = e.matmul(bx[:], lhsT=onesb[:], rhs=ys[cc][2], start=(cc == 0), stop=(cc == 1))
                    return i
                S.op("pe", mmx, reads=[TONE, ys[0][3], ys[1][3]], writes=[tbx])
                mean, tmean = rot_tf.next()
                S.op("dve", lambda e, mean=mean, bm=bm: e.tensor_scalar(out=mean, in0=bm[:], scalar1=1.0 / 256, scalar2=None,
                                                                       op0=ALU.mult), reads=[tbm], writes=[tmean])
                var, tvar = rot_tf.next()
                S.op("dve", lambda e, var=var, mean=mean: e.tensor_tensor(out=var, in0=mean, in1=mean, op=ALU.mult),
                     reads=[tmean], writes=[tvar])
                S.op("dve", lambda e, var=var, bx=bx: e.scalar_tensor_tensor(out=var, in0=bx[:], scalar=1.0 / 256, in1=var,
                                                                            op0=ALU.mult, op1=ALU.subtract),
                     reads=[tbx, tvar], writes=[tvar])
                cst[j]["mv"] = (mean, tmean, var, tvar)

            def convB2(j):
                ys = cst[j]["ys"]
                mean, tmean, var, tvar = cst[j]["mv"]
                S.op("act", lambda e: e.activation(out=var, in_=var, func=AF.Ln, bias=EPS, scale=1.0),
                     reads=[tvar], writes=[tvar])
                S.op("act", lambda e: e.activation(out=var, in_=var, func=AF.Exp, scale=-0.5),
                     reads=[tvar], writes=[tvar])
                css = []
                for cc in range(2):
                    y32, ty32 = ys[cc][0], ys[cc][1]
                    S.op("dve", lambda e, y32=y32: e.tensor_tensor(out=y32, in0=y32, in1=mean, op=ALU.subtract),
                         reads=[ty32, tmean], writes=[ty32])
                    S.op("dve", lambda e, y32=y32: e.tensor_tensor(out=y32, in0=y32, in1=var, op=ALU.mult),
                         reads=[ty32, tvar], writes=[ty32])
                    cs, tcs = css_slots[j % 2][cc]
                    S.op("act", lambda e, cs=cs, y32=y32, cc=cc: e.activation(out=cs, in_=y32, func=AF.Silu,
                                                                             bias=vcol(l, 82 + cc), scale=vcol(l, 80 + cc)),
                         reads=[ty32, TVEC], writes=[tcs])
                    css.append((cs, tcs))
                cst[j]["css"] = css

            def convC(j):
                css = cst[j]["css"]
                for oc in range(2):
                    bo, tbo = rot_main.next()

                    def mmo(e, bo=bo, oc=oc):
                        for cc in range(2):
                            i = e.matmul(bo[:], lhsT=pw[:, cc, oc * 128:(oc + 1) * 128], rhs=css[cc][0],
                                         start=(cc == 0), stop=(cc == 1))
                        return i
                    S.op("pe", mmo, reads=[TPW, css[0][1], css[1][1]], writes=[tbo])
                    S.op("act", lambda e, bo=bo, oc=oc, j=j: e.activation(out=mixT[:, oc, chs(j)], in_=bo[:], func=AF.Identity,
                                                                         bias=vcol(l, 84 + oc), scale=1.0),
                         reads=[tbo, TVEC], writes=[TM[oc][j]])

            for i_ in range(NCH + 1):
                if i_ < NCH:
                    convA(i_)
                if 0 <= i_ - 1 < NCH:
                    convB2(i_ - 1)
                if i_ < NCH:
                    convB(i_)
                if 0 <= i_ - 1 < NCH:
                    convC(i_ - 1)
                    if dbg not in ("convonly", "poolonly"):
                        wout_apply(i_ - 1)
            if dbg in ("convonly", "poolonly"):
                dump(dbg, mixT[:], [t for r in TM for t in r], bf=True)
                return finish()
            if dbg == "convpool" and l == dbg_layer[0]:
                dump("convpool", xT[:], [t for r in TX for t in r])
                return finish()
            S.barrier()
            load_wo(l, 1)
            for m in range(2):
                S.dma("pool", lambda e, m=m: e.dma_start(out=QT[64:68, m, :], in_=aug_d[0]), writes=[TQA])
                S.dma("pool", lambda e, m=m: e.dma_start(out=KT[64:68, m, :], in_=aug_d[1]), writes=[TKA])
            S.op("pool", lambda e: e.memset(V[:, :, 128:129], 1.0), writes=[TVO])
            carry = []

            def finalize2(w_, tw_, rr, trr):
                S.op("act", lambda e: e.activation(out=rr[:, 4:5], in_=rr[:, 3:4], func=AF.Ln, bias=EPS, scale=1.0 / 128),
                     reads=[trr], writes=[trr])
                S.op("act", lambda e: e.activation(out=rr[:, 5:6], in_=rr[:, 4:5], func=AF.Exp, scale=-0.5),
                     reads=[trr], writes=[trr])
                S.op("dve", lambda e: e.tensor_scalar(out=w_[:, 129:257], in0=w_[:, 0:128], scalar1=rr[:, 5:6], scalar2=None,
                                                      op0=ALU.mult), reads=[tw_, trr], writes=[tw_])

            def emit_carry():
                for (jq, w_, tw_, qcc, rr, trr, hh) in carry:
                    finalize2(w_, tw_, rr, trr)
                for (jq, w_, tw_, qcc, rr, trr, hh) in carry:
                    S.op("pe", lambda e, jq=jq, w_=w_: e.transpose(b7[:, jq * 128:(jq + 1) * 128], w_[:, 129:257], identf[:]),
                         reads=[tw_, TIDF], writes=[TB7])
                    if jq == 3:
                        S.op("act", lambda e, qcc=qcc, hh=hh: e.activation(out=mixT[:, hh, chs(qcc)], in_=b7[:], func=AF.Identity,
                                                                          scale=cols[:, 7:8]), reads=[TB7, TCOL], writes=[TM[hh][qcc]])
                carry.clear()

            emit_carry_prev = emit_carry
            for h in range(4):
                slot = h % 2
                def qk_post(zb, tzb, qk, m, j, h=h):
                    sq, tsq = rot_tb.next()
                    S.op("act", lambda e: e.activation(out=sq[0:64, :], in_=zb[0:64, :], func=AF.Square),
                         reads=[tzb], writes=[tsq])
                    bs_, tbs_ = rot_ss.next()
                    S.op("pe", lambda e: e.matmul(bs_[0:64, :], lhsT=onesb[0:64, 0:64], rhs=sq[0:64, :],
                                                  start=True, stop=True), reads=[tsq, TONE], writes=[tbs_])
                    r, tr_ = rstd_from(bs_, tbs_, 64, 1.0 / 64)
                    dst = (QT if qk == 0 else KT)[0:64, m, chs(j)]
                    dt_ = (TQ if qk == 0 else TK)[m][j]
                    mc = cols[0:64, h:h + 1] if qk == 0 else cols[0:64, 4:5]
                    S.op("dve", lambda e: e.scalar_tensor_tensor(
                        out=dst, in0=zb[0:64, :], scalar=mc, in1=r[0:64, :], op0=ALU.mult, op1=ALU.mult),
                        reads=[tzb, tr_, TCOL], writes=[dt_])
                qk_pend = None
                ngrp = 0
                for j in range(NCH):
                    for qk in range(2):
                        for m in range(2):
                            zb, tzb = rot_main.next()
                            co = qk * 128 + m * 64

                            def mmz(e, zb=zb, co=co, j=j):
                                for c in range(8):
                                    i = e.matmul(zb[0:64, :], lhsT=wi[:, slot, c, co:co + 64], rhs=hT[:, c, chs(j)],
                                                 start=(c == 0), stop=(c == 7))
                                return i
                            S.op("pe", mmz, reads=[TWI[slot]] + [TH[c][j] for c in range(8)], writes=[tzb])
                            if qk_pend is not None:
                                qk_post(*qk_pend)
                            qk_pend = (zb, tzb, qk, m, j)
                            ngrp += 1
                            if ngrp == 2 and carry:
                                emit_carry_prev()
                    vb, tvb = rot_main.next()

                    def mmv2(e, vb=vb, j=j):
                        for tt in range(4):
                            for c in range(8):
                                i = e.matmul(vb[:, tt * 128:(tt + 1) * 128], lhsT=hT[:, c, j * CH + tt * 128:j * CH + (tt + 1) * 128],
                                             rhs=wi[:, slot, c, 256:384], start=(c == 0), stop=(c == 7))
                        return i
                    S.op("pe", mmv2, reads=[TWI[slot]] + [TH[c][j] for c in range(8)], writes=[tvb])
                    S.op("dve", lambda e, vb=vb, j=j: e.tensor_copy(V[:, 4 * j:4 * j + 4, 0:128],
                                                                   vb[:].rearrange("p (k d) -> p k d", k=4)),
                         reads=[tvb], writes=[TV[j]])
                qk_post(*qk_pend)
                if h < 3:
                    load_wi(l, 4 + h, (h + 1) % 2)
                if dbg == "qkv" and l == dbg_layer[0] and h == 0:
                    dump("qkv", A[:, 8192:16384].rearrange("p (c t) -> p c t", c=4), [t for r in TQ + TK for t in r] + [TQA, TKA], bf=True)
                    return finish()
                for qc in range(NCH):
                    nkb = 4 * qc + 4
                    pend = None
                    fin_pending = []
                    chain_pend = None

                    def emit_pv(kb, pts, c0):
                        jq_min = max(0, kb - 4 * qc)
                        for m in range(2):
                            pt, tpt = pts[m]
                            for jq in range(jq_min, 4):
                                a = accap[m][jq]
                                S.op("pe", lambda e, a=a, pt=pt, jq=jq, kb=kb, c0=c0: e.matmul(
                                    a, lhsT=pt[:, jq * 128 - c0:jq * 128 - c0 + 128], rhs=V[:, kb, 0:129],
                                    start=(kb == 0 and m == 0), stop=(kb == 4 * qc + jq),
                                    skip_group_check=True),
                                    reads=[tpt, TV[kb // 4], TVO], writes=[TACC[m][jq]])

                    def fin_copy(jq):
                        bkq = banks[3 + jq]
                        rr, trr = rot_sc.next()
                        w_, tw_ = y32_slots[jq // 2][jq % 2]
                        S.op("dve", lambda e: e.tensor_copy(w_[:, 0:258], bkq[:, 0:258]), reads=[TB[3 + jq]], writes=[tw_])
                        return (jq, w_, tw_, rr, trr)

                    def fin_chain(args, h=h):
                        jq, w_, tw_, rr, trr = args
                        S.op("dve", lambda e: e.reciprocal(out=rr[:, 0:1], in_=w_[:, 128:129]), reads=[tw_], writes=[trr])
                        S.op("dve", lambda e: e.reciprocal(out=rr[:, 1:2], in_=w_[:, 257:258]), reads=[tw_], writes=[trr])
                        S.op("dve", lambda e: e.tensor_tensor(out=rr[:, 2:3], in0=rr[:, 1:2], in1=cols[:, 8:9], op=ALU.mult),
                             reads=[trr, TCOL], writes=[trr])
                        S.op("dve", lambda e: e.tensor_scalar(out=w_[:, 129:257], in0=w_[:, 129:257], scalar1=rr[:, 2:3], scalar2=None,
                                                              op0=ALU.mult), reads=[trr, tw_], writes=[tw_])
                        S.op("dve", lambda e: e.scalar_tensor_tensor(out=w_[:, 0:128], in0=w_[:, 0:128], scalar=rr[:, 0:1],
                                                                     in1=w_[:, 129:257], op0=ALU.mult, op1=ALU.add),
                             reads=[trr, tw_], writes=[tw_])
                        S.op("dve", lambda e: e.tensor_tensor(out=w_[:, 258:386], in0=w_[:, 0:128], in1=w_[:, 0:128],
                                                              op=ALU.mult), reads=[tw_], writes=[tw_])
                        S.op("dve", lambda e: e.reduce_sum(out=rr[:, 3:4], in_=w_[:, 258:386], axis=AX.X), reads=[tw_, trr], writes=[trr])
                        return (jq, w_, tw_, qc, rr, trr, h)

                    for kb in range(nkb):
                        jq_min = max(0, kb - 4 * qc)
                        c0 = jq_min * 128
                        pts = []
                        if kb == 2 and carry:
                            emit_carry()
                        for m in range(2):
                            st, tst = rot_st.next()
                            rd = [TK[m][kb // 4], TKA, TQ[m][qc], TQA]
                            if kb >= 4 * qc:
                                def mmd(e, st=st, m=m, kb=kb, c0=c0):
                                    e.matmul(st[:, c0:c0 + 128], lhsT=identb[:], rhs=maskb[:], start=True, stop=False)
                                    i = e.matmul(st[:, c0:c0 + 128], lhsT=KT[0:68, m, kb * 128:(kb + 1) * 128],
                                                 rhs=QT[0:68, m, qc * CH + c0:qc * CH + c0 + 128], start=False, stop=True)
                                    if c0 + 128 < CH:
                                        i = e.matmul(st[:, c0 + 128:CH], lhsT=KT[0:68, m, kb * 128:(kb + 1) * 128],
                                                     rhs=QT[0:68, m, qc * CH + c0 + 128:(qc + 1) * CH], start=True, stop=True)
                                    return i
                                S.op("pe", mmd, reads=rd + [TIDB, TMSK], writes=[tst])
                            else:
                                S.op("pe", lambda e, st=st, m=m, kb=kb: e.matmul(
                                    st[:], lhsT=KT[0:68, m, kb * 128:(kb + 1) * 128], rhs=QT[0:68, m, chs(qc)],
                                    start=True, stop=True), reads=rd, writes=[tst])
                            pt, tpt = rot_tb.next()
                            S.op("act", lambda e, pt=pt, st=st, c0=c0: e.activation(out=pt[:, 0:CH - c0], in_=st[:, c0:CH],
                                                                                   func=AF.Exp, scale=SLOPES[h]),
                                 reads=[tst], writes=[tpt])
                            pts.append((pt, tpt))
                        if pend is not None:
                            emit_pv(*pend)
                            kbp = pend[0]
                            if kbp >= 4 * qc:
                                cnew = fin_copy(kbp - 4 * qc)
                                if chain_pend is not None:
                                    fin_pending.append(fin_chain(chain_pend))
                                chain_pend = cnew
                        pend = (kb, pts, c0)
                    emit_pv(*pend)
                    cnew = fin_copy(3)
                    if chain_pend is not None:
                        fin_pending.append(fin_chain(chain_pend))
                    fin_pending.append(fin_chain(cnew))
                    chain_pend = None
                    assert not carry
                    carry.extend(fin_pending)
                    fin_pending.clear()
                if h == 3 or dbg == "attn":
                    emit_carry()
                if dbg == "attn" and l == dbg_layer[0] and h == 0:
                    dump("attn", mixT[:, 0:1, :], [t for t in TM[0]], bf=True)
                    return finish()
            for j in range(NCH):
                wout_apply(j)
            if dbg == "mixer" and l == dbg_layer[0]:
                dump("mixer", xT[:], [t for r in TX for t in r])
                return finish()
            S.barrier()
            def load_expert(e_, slot):
                S.dma("pool", lambda e: e.dma_start(out=gu[slot][:, :, 0:256],
                                                    in_=w_gate[l, e_].rearrange("(c p) n -> p c n", p=128)), writes=[TGa[slot]])
                S.dma("pool", lambda e: e.dma_start(out=gu[slot][:, :, 256:512],
                                                    in_=w_up[l, e_].rearrange("(c p) n -> p c n", p=128)), writes=[TUa[slot]])
                S.dma("pool", lambda e: e.dma_start(out=dn[slot][:],
                                                    in_=w_down[l, e_].rearrange("(c p) n -> p c n", p=128)), writes=[TDa[slot]])
            load_expert(0, 0)
            load_expert(1, 1)
            gt_pend = [None]
            for j in range(NCH):
                r, tr_ = norm(l, 8, j, no_dve=True)
                bk, tbk = rot_main.next()

                def mmr(e, bk=bk, j=j, r=r):
                    for tt in range(4):
                        for c in range(8):
                            e.matmul(bk[:, tt * 32:tt * 32 + 20], lhsT=xT[:, c, j * CH + tt * 128:j * CH + (tt + 1) * 128],
                                     rhs=wrg[:, c, :], start=(c == 0), stop=(c == 7))
                        i = e.matmul(bk[:, tt * 32 + 20:tt * 32 + 21], lhsT=r[0:1, tt * 128:(tt + 1) * 128],
                                     rhs=identf[0:1, 0:1], start=True, stop=True)
                    return i
                S.op("pe", mmr, reads=[TX[c][j] for c in range(8)] + [TWRG, tr_, TIDF], writes=[tbk])
                bv = bk[:, 0:128].rearrange("p (t n) -> p t n", t=4)
                rd, wrt = [TR, TVEC], [TR]

                def R(fn, extra=()):
                    S.op("dve", fn, reads=rd + list(extra), writes=wrt)
                R(lambda e, bv=bv: e.tensor_copy(rs4[:, 0, :], bv[:, :, 20]), [tbk])
                for tt in range(4):
                    R(lambda e, bv=bv, tt=tt: e.scalar_tensor_tensor(out=rL[:, tt, :], in0=bv[:, tt, 0:20], scalar=rs4[:, 0, tt:tt + 1],
                                                                    in1=vcol(l, 347, 20), op0=ALU.mult, op1=ALU.add), [tbk])
                Lg = rL[:, :, 0:4]

                def bc(ap2):
                    return ap2.unsqueeze(2).to_broadcast([128, 4, 4])
                R(lambda e: e.tensor_reduce(out=rs4[:, 1, :], in_=Lg, axis=AX.X, op=ALU.max))
                R(lambda e: e.tensor_tensor(out=rA[:], in0=Lg, in1=bc(rs4[:, 1, :]), op=ALU.is_equal))
                R(lambda e: e.tensor_tensor(out=rB[:], in0=Lg, in1=bc(rs4[:, 1, :]), op=ALU.subtract))
                S.op("act", lambda e: e.activation(out=rB[:], in_=rB[:], func=AF.Exp), reads=rd, writes=wrt)
                R(lambda e: e.reduce_sum(out=rs4[:, 2, :], in_=rB[:], axis=AX.X))
                R(lambda e: e.reciprocal(out=rs4[:, 3, :], in_=rs4[:, 2, :]))
                for g in range(4):
                    le_g = rL[:, :, 4 + 4 * g:8 + 4 * g]
                    if g == 0:
                        R(lambda e, le_g=le_g: e.tensor_tensor(out=rC[:], in0=le_g, in1=bc(rA[:, :, 0]), op=ALU.mult))
                    else:
                        R(lambda e, le_g=le_g, g=g: e.tensor_tensor(out=rD[:], in0=le_g, in1=bc(rA[:, :, g]), op=ALU.mult))
                        R(lambda e: e.tensor_tensor(out=rC[:], in0=rC[:], in1=rD[:], op=ALU.add))
                R(lambda e: e.tensor_reduce(out=rs4[:, 4, :], in_=rC[:], axis=AX.X, op=ALU.max))
                R(lambda e: e.tensor_tensor(out=rD[:], in0=rC[:], in1=bc(rs4[:, 4, :]), op=ALU.is_equal))
                R(lambda e: e.scalar_tensor_tensor(out=rE[:], in0=rD[:], scalar=-1e30, in1=rC[:], op0=ALU.mult, op1=ALU.add))
                R(lambda e: e.tensor_reduce(out=rs4[:, 5, :], in_=rE[:], axis=AX.X, op=ALU.max))
                R(lambda e: e.tensor_tensor(out=rE[:], in0=rE[:], in1=bc(rs4[:, 5, :]), op=ALU.is_equal))
                R(lambda e: e.tensor_tensor(out=rs4[:, 6, :], in0=rs4[:, 5, :], in1=rs4[:, 4, :], op=ALU.subtract))
                S.op("act", lambda e: e.activation(out=rs4[:, 6, :], in_=rs4[:, 6, :], func=AF.Exp), reads=rd, writes=wrt)
                R(lambda e: e.tensor_scalar(out=rs4[:, 7, :], in0=rs4[:, 6, :], scalar1=1.0, scalar2=None, op0=ALU.add))
                R(lambda e: e.reciprocal(out=rs4[:, 7, :], in_=rs4[:, 7, :]))
                R(lambda e: e.tensor_tensor(out=rs4[:, 8, :], in0=rs4[:, 6, :], in1=rs4[:, 7, :], op=ALU.mult))
                R(lambda e: e.tensor_tensor(out=rs4[:, 7, :], in0=rs4[:, 7, :], in1=rs4[:, 3, :], op=ALU.mult))
                R(lambda e: e.tensor_tensor(out=rs4[:, 8, :], in0=rs4[:, 8, :], in1=rs4[:, 3, :], op=ALU.mult))
                R(lambda e: e.tensor_tensor(out=rD[:], in0=rD[:], in1=bc(rs4[:, 7, :]), op=ALU.mult))
                R(lambda e: e.tensor_tensor(out=rE[:], in0=rE[:], in1=bc(rs4[:, 8, :]), op=ALU.mult))
                R(lambda e: e.tensor_tensor(out=rD[:], in0=rD[:], in1=rE[:], op=ALU.add))
                for g in range(4):
                    R(lambda e, g=g: e.tensor_tensor(out=rG[:, :, 4 * g:4 * g + 4], in0=rD[:], in1=bc(rA[:, :, g]), op=ALU.mult))
                R(lambda e: e.tensor_copy(rGb[:], rG[:]))
                gpar = j % 2
                S.op("dve", lambda e, gpar=gpar: e.tensor_copy(rG32[:, gpar, :, 0:16], rGb[:]), reads=rd, writes=[TR, TRG[gpar]])
                S.op("dve", lambda e, gpar=gpar: e.tensor_tensor(out=rG32[:, gpar, :, 16:32], in0=rG[:], in1=rG32[:, gpar, :, 0:16],
                                                                op=ALU.subtract), reads=rd + [TRG[gpar]], writes=[TR, TRG[gpar]])

                def gate_transposes(j=j, gpar=gpar):
                    bt, tbt = rot_main.next()

                    def trg(e):
                        for tt in range(4):
                            i = e.transpose(bt[0:32, tt * 128:(tt + 1) * 128], rG32[:, gpar, tt, :], identf[:])
                        return i
                    S.op("pe", trg, reads=[TRG[gpar], TIDF], writes=[tbt])
                    S.op("act", lambda e: e.activation(out=gT[:, chs(j)], in_=bt[0:32, :], func=AF.Copy),
                         reads=[tbt], writes=[TGT[j]])
                if gt_pend[0] is not None:
                    gt_pend[0]()
                gt_pend[0] = gate_transposes
            gt_pend[0]()
            gt_pend[0] = None
            if dbg == "router" and l == dbg_layer[0]:
                dump("router", gT[:].rearrange("p (c t) -> p c t", c=1), TGT, bf=True)
                return finish()
            pend_down = None

            def emit_down(e_, slot, j, par):
                for fc in range(8):
                    by, tby = rot_y.next()

                    def mmd2(e, by=by, fc=fc):
                        for dc in range(2):
                            i = e.matmul(by[:], lhsT=dn[slot][:, dc, fc * 128:(fc + 1) * 128], rhs=actT[:, par, dc, :],
                                         start=(dc == 0), stop=(dc == 1))
                        return i
                    S.op("pe", mmd2, reads=[TDa[slot], TACT[par][0], TACT[par][1]], writes=[tby])
                    S.op("dve", lambda e, by=by, fc=fc: e.tensor_tensor(out=xT[:, fc, chs(j)], in0=by[:], in1=xT[:, fc, chs(j)],
                                                                        op=ALU.add), reads=[tby, TX[fc][j]], writes=[TX[fc][j]])
            items = [(e_, j) for e_ in range(16) for j in range(NCH)]
            gstate = {}

            def gate_stage1(idx):
                e_, j = items[idx]
                gm, tgm = rot_tb.next()
                S.op("dve", lambda e: e.tensor_scalar(out=gm[0:32, :], in0=gT[0:32, chs(j)], scalar1=selc[:, e_:e_ + 1],
                                                      scalar2=None, op0=ALU.mult), reads=[TSEL, TGT[j]], writes=[tgm])
                gstate[idx] = (gm, tgm)

            def gate_stage2(idx):
                gm, tgm = gstate.pop(idx)
                S.op("pe", lambda e: e.matmul(b7[:], lhsT=onesb[0:32, :], rhs=gm[0:32, :],
                                              start=True, stop=True), reads=[TONE, tgm], writes=[TB7])
                gb, tgb = rot_tf.next()
                S.op("act", lambda e: e.activation(out=gb, in_=b7[:], func=AF.Copy), reads=[TB7], writes=[tgb])
                gstate[("gb", idx)] = (gb, tgb)

            gate_stage1(0)
            gate_stage2(0)
            item = 0
            for e_ in range(16):
                slot = e_ % 2
                for j in range(NCH):
                    par = item % 2
                    idx = item
                    item += 1
                    gb, tgb = gstate.pop(("gb", idx))
                    if e_ == 12 and j == 0 and dbg is None:
                        nl = l + 1 if l + 1 < n_layer else (0 if s + 1 < n_seq else None)
                        if nl is not None:
                            load_wi(nl, 1, 1)
                            load_wi(nl, 0, 0)
                            wi_prefetched[0] = True
                    if idx + 1 < len(items):
                        gate_stage1(idx + 1)
                    for dc in range(2):
                        bA, tbA = rot_main.next()
                        bB, tbB = rot_main.next()

                        def mmgu(e, bA=bA, bB=bB, dc=dc, j=j, slot=slot):
                            for c in range(8):
                                e.matmul(bA[:], lhsT=gu[slot][:, c, dc * 128:(dc + 1) * 128], rhs=hT[:, c, chs(j)],
                                         start=(c == 0), stop=(c == 7))
                            for c in range(8):
                                i = e.matmul(bB[:], lhsT=gu[slot][:, c, 256 + dc * 128:256 + (dc + 1) * 128], rhs=hT[:, c, chs(j)],
                                             start=(c == 0), stop=(c == 7))
                            return i
                        S.op("pe", mmgu, reads=[TGa[slot], TUa[slot]] + [TH[c][j] for c in range(8)], writes=[tbA, tbB])
                        sg_, tsg = rot_tf.next()
                        S.op("act", lambda e, sg_=sg_, bA=bA: e.activation(out=sg_, in_=bA[:], func=AF.Silu), reads=[tbA], writes=[tsg])
                        S.op("dve", lambda e, sg_=sg_, bB=bB: e.tensor_tensor(out=sg_, in0=bB[:], in1=sg_, op=ALU.mult),
                             reads=[tbB, tsg], writes=[tsg])
                        S.op("pool", lambda e, sg_=sg_, gb=gb, par=par, dc=dc: e.tensor_tensor(out=actT[:, par, dc, :], in0=sg_, in1=gb,
                                                                                             op=ALU.mult),
                             reads=[tsg, tgb], writes=[TACT[par][dc]])
                    if idx + 1 < len(items):
                        gate_stage2(idx + 1)
                    if pend_down is not None:
                        emit_down(*pend_down)
                    pend_down = (e_, slot, j, par)
                    if j == NCH - 1 and e_ + 2 < 16:
                        emit_down(*pend_down)
                        pend_down = None
                        load_expert(e_ + 2, slot)
            if pend_down is not None:
                emit_down(*pend_down)
            if dbg == "layer" and l == dbg_layer[0]:
                dump("layer", xT[:], [t for r in TX for t in r])
                return finish()
            S.barrier()
        if s + 1 < n_seq:
            for tt in range(16):
                store_x(s, [tt])
                load_x(s + 1, [tt])
        else:
            store_x(s)
    return finish()


dbg_layer = [0]
aug_b = None


def _build_with_aug(n_seq=2, n_layer=2, dbg=None, layer=0):
    global aug_b
    dbg_layer[0] = layer
    return build(n_seq, n_layer, dbg)


def host_consts():
    t = np.arange(SEQ)
    aug = np.zeros((2, 4, SEQ), np.float32)
    aug[0, 0] = -128.0 * (t // 128)
    aug[0, 1] = -(t % 128).astype(np.float32)
    aug[0, 2] = 1.0
    aug[0, 3] = 1.0
    aug[1, 0] = 1.0
    aug[1, 1] = 1.0
    aug[1, 2] = 128.0 * (t // 128)
    aug[1, 3] = (t % 128).astype(np.float32)
    k = np.arange(128)[:, None]
    q = np.arange(128)[None, :]
    cmask = np.where(k > q, NEGMASK, 0.0).astype(np.float32)
    sel = np.zeros((32, 16), np.float32)
    for kk in range(32):
        sel[kk, kk % 16] = 1.0
    ident = np.eye(128, dtype=np.float32)
    return aug, cmask, sel, ident


def pack_vecs(inp):
    v = np.zeros((128, NV), np.float32)
    for l in range(2):
        b = l * LV
        v[:, b + 0:b + 8] = inp["attn_norm_g"][l].reshape(8, 128).T
        v[:, b + 8:b + 16] = inp["ffn_norm_g"][l].reshape(8, 128).T
        cw = inp["conv_w"][l]
        for cc in range(2):
            v[:, b + 16 + cc * 31:b + 16 + (cc + 1) * 31] = cw[:, cc * 128:(cc + 1) * 128].T
        v[:, b + 78:b + 80] = inp["conv_b"][l].reshape(2, 128).T
        v[:, b + 80:b + 82] = inp["conv_ln_g"][l].reshape(2, 128).T
        v[:, b + 82:b + 84] = inp["conv_ln_b"][l].reshape(2, 128).T
        v[:, b + 84:b + 86] = inp["conv_pw_b"][l].reshape(2, 128).T
        v[:, b + 86:b + 88] = inp["pool_scale"][l].reshape(2, 128).T
        v[0:64, b + 88] = inp["q_norm_g"][l]
        v[0:64, b + 89] = inp["k_norm_g"][l]
        v[:, b + 90] = inp["attn_sub_norm_g"][l]
        for i, nm in enumerate(("lambda_q1", "lambda_k1", "lambda_q2", "lambda_k2")):
            v[:, b + 91 + 64 * i:b + 91 + 64 * (i + 1)] = inp[nm][l][None, :]
        v[:, b + 347:b + 351] = inp["router_g_b"][l][None, :]
        v[:, b + 351:b + 367] = inp["router_e_b"][l][None, :]
    b = 2 * LV
    wins = np.array([[2, 8], [4, 16]], np.float32)
    for cc in range(2):
        v[0:64, b + cc] = 1.0 / wins[0, cc]
        v[64:128, b + cc] = 1.0 / wins[1, cc]
        for tt in range(16):
            v[0:64, b + 2 + cc * 16 + tt] = 1.0 / min(tt + 1, wins[0, cc])
            v[64:128, b + 2 + cc * 16 + tt] = 1.0 / min(tt + 1, wins[1, cc])
    return v


def pack_wr(inp):
    w = np.concatenate([inp["router_g_w"], inp["router_e_w"]], axis=2)
    w = w.reshape(2, 8, 128, 20).transpose(2, 0, 1, 3)
    return np.ascontiguousarray(w)


_NC_CACHE = {}


def make_in_maps(inp, ncores=8):
    inp = {k: np.asarray(v) for k, v in inp.items()}
    aug, cmask, sel, ident = host_consts()
    shared = {
        "w_in": inp["w_in"], "w_out": inp["w_out"], "conv_pw_w": inp["conv_pw_w"], "pool_w": inp["pool_w"],
        "w_gate": inp["w_gate"], "w_up": inp["w_up"], "w_down": inp["w_down"],
        "wr": pack_wr(inp), "vecs": pack_vecs(inp), "aug": aug, "cmask": cmask, "sel": sel, "ident": ident,
    }
    maps = []
    for c in range(ncores):
        m = dict(shared)
        m["x"] = np.ascontiguousarray(inp["x"][2 * c:2 * c + 2])
        maps.append(m)
    return maps


def kernel(**inputs):
    global aug_b
    if "nc" not in _NC_CACHE:
        _NC_CACHE["nc"] = _build_with_aug()
    nc = _NC_CACHE["nc"]
    maps = make_in_maps(inputs)
    res = run_bass_kernel_spmd(nc, maps, core_ids=list(range(8)))
    outs = [np.asarray(r["out"]) for r in res.results]
    return np.concatenate(outs, axis=0).astype(np.float32)
```
